# Optimizing a Trainium2 kernel written in Bass

```python
import jax, jax.numpy as jnp
from jax import lax
import numpy as np

D_MODEL = 2048
BATCH = 16
SEQ = 2048
DEPTH = 2
DEC_BATCH = 16
DEC_SEQ = 16
PAST_LEN = 1024

CHUNK = 64
N_MIXERS = 2
N_RGLRU_LAYERS = (DEPTH + 1) // 2
N_CONV_LAYERS = DEPTH // 2
N_DENSE_LAYERS = (DEPTH + 1) // 2
N_MOE_LAYERS = DEPTH // 2
LRU_WIDTH = D_MODEL
LRU_HEADS = 8
LRU_BLOCK = LRU_WIDTH // LRU_HEADS
LRU_CONV_W = 4
LRU_C = 8.0
CM_KERNEL = 31
D_FF = 5632
N_EXPERTS = 8
TOP_K = 2
MOE_D_FF = 2816
PLE_DIM = 256
LN_EPS = 1e-5
DEEPNORM_ALPHA = (2.0 * DEPTH) ** 0.25
DEEPNORM_BETA = (8.0 * DEPTH) ** -0.25

kernel_name = 'hybrid_rglru_conformer_stream_step'


def layer_norm(x, g, b):
    xf = x.astype(jnp.float32)
    mu = jnp.mean(xf, axis=-1, keepdims=True)
    var = jnp.mean(jnp.square(xf - mu), axis=-1, keepdims=True)
    y = (xf - mu) * lax.rsqrt(var + LN_EPS)
    return (y * g.astype(jnp.float32) + b.astype(jnp.float32)).astype(x.dtype)


def causal_depthwise_conv(x, buf, w, b):
    k = w.shape[0]
    xp = jnp.concatenate([buf.astype(x.dtype), x], axis=1)
    y = lax.conv_general_dilated(xp, w[:, None, :].astype(x.dtype), window_strides=(1,),
                                 padding='VALID', dimension_numbers=('NWC', 'WIO', 'NWC'),
                                 feature_group_count=x.shape[-1])
    return y + b, xp[:, -(k - 1):]


def rglru_block(u, conv_buf, h0, w_in, conv_w, conv_b, w_a, b_a, w_x, b_x, lam, w_out):
    bsz, t, _ = u.shape
    gate_br, rec_br = jnp.split(u @ w_in, 2, axis=-1)
    xc, new_buf = causal_depthwise_conv(rec_br, conv_buf, conv_w, conv_b)
    xh = xc.reshape(bsz, t, LRU_HEADS, LRU_BLOCK)
    r = jax.nn.sigmoid(jnp.einsum('bthi,hij->bthj', xh, w_a).reshape(bsz, t, LRU_WIDTH) + b_a)
    ig = jax.nn.sigmoid(jnp.einsum('bthi,hij->bthj', xh, w_x).reshape(bsz, t, LRU_WIDTH) + b_x)
    log_a = (-LRU_C * r.astype(jnp.float32)) * jax.nn.softplus(-lam.astype(jnp.float32))
    a = jnp.exp(log_a)
    mult = jnp.sqrt(-jnp.expm1(2.0 * log_a))
    bterm = mult * (ig * xc).astype(jnp.float32)
    bterm = bterm.at[:, 0].add(a[:, 0] * h0.astype(jnp.float32))

    def combine(left, right):
        a1, b1 = left
        a2, b2 = right
        return a1 * a2, a2 * b1 + b2

    _, h = lax.associative_scan(combine, (a, bterm), axis=1)
    y = (h.astype(u.dtype) * jax.nn.gelu(gate_br)) @ w_out
    return y, new_buf, h[:, -1].astype(u.dtype)


def conv_module(u, buf, w_pw1, b_pw1, dw_w, dw_b, ln_g, ln_b, w_pw2, b_pw2):
    val, gate = jnp.split(u @ w_pw1 + b_pw1, 2, axis=-1)
    v = val * jax.nn.sigmoid(gate)
    y, new_buf = causal_depthwise_conv(v, buf, dw_w, dw_b)
    y = jax.nn.silu(layer_norm(y, ln_g, ln_b))
    return y @ w_pw2 + b_pw2, new_buf


def swiglu(u, w_in, w_out):
    g, up = jnp.split(u @ w_in, 2, axis=-1)
    return (jax.nn.silu(g) * up) @ w_out


def moe_swiglu(u, w_router, w_in, w_out):
    probs = jax.nn.softmax((u @ w_router).astype(jnp.float32), axis=-1)
    top_p, top_i = lax.top_k(probs, TOP_K)
    top_p = top_p / jnp.sum(top_p, axis=-1, keepdims=True)
    gates = jnp.sum(jax.nn.one_hot(top_i, N_EXPERTS, dtype=jnp.float32) * top_p[..., None], axis=-2)
    out = jnp.zeros_like(u)
    for e in range(N_EXPERTS):
        out = out + gates[..., e:e + 1].astype(u.dtype) * swiglu(u, w_in[e], w_out[e])
    return out


def trunk(x, p, lru_conv, lru_h, cm_conv,
          rglru_w_in, rglru_conv_w, rglru_conv_b, rglru_w_a, rglru_b_a, rglru_w_x, rglru_b_x,
          rglru_lambda, rglru_w_out,
          cm_w_pw1, cm_b_pw1, cm_dw_w, cm_dw_b, cm_ln_g, cm_ln_b, cm_w_pw2, cm_b_pw2,
          ffn_w_in, ffn_w_out, moe_w_router, moe_w_in, moe_w_out,
          ln_mix_g, ln_mix_b, ln_ffn_g, ln_ffn_b, ple_w_proj, ple_w_gate, ple_b_gate):
    new_lru_conv, new_lru_h, new_cm_conv = [], [], []
    for i in range(DEPTH):
        j = i // N_MIXERS
        if i % N_MIXERS == 0:
            mix, buf, h = rglru_block(x, lru_conv[j], lru_h[j], rglru_w_in[j], rglru_conv_w[j],
                                      rglru_conv_b[j], rglru_w_a[j], rglru_b_a[j], rglru_w_x[j],
                                      rglru_b_x[j], rglru_lambda[j], rglru_w_out[j])
            new_lru_conv.append(buf)
            new_lru_h.append(h)
        else:
            mix, buf = conv_module(x, cm_conv[j], cm_w_pw1[j], cm_b_pw1[j], cm_dw_w[j], cm_dw_b[j],
                                   cm_ln_g[j], cm_ln_b[j], cm_w_pw2[j], cm_b_pw2[j])
            new_cm_conv.append(buf)
        x = layer_norm(DEEPNORM_ALPHA * x + mix, ln_mix_g[i], ln_mix_b[i])
        k = i // 2
        if i % 2 == 0:
            ff = swiglu(x, ffn_w_in[k], ffn_w_out[k])
        else:
            ff = moe_swiglu(x, moe_w_router[k], moe_w_in[k], moe_w_out[k])
        x = layer_norm(DEEPNORM_ALPHA * x + ff, ln_ffn_g[i], ln_ffn_b[i])
        x = x + jax.nn.sigmoid(x @ ple_w_gate[i] + ple_b_gate[i]) * (p[i] @ ple_w_proj[i])
    return x, jnp.stack(new_lru_conv), jnp.stack(new_lru_h), jnp.stack(new_cm_conv)


def setup_inputs(seed: int = 0) -> dict:
    key = jax.random.key(seed)
    keys = jax.random.split(key, 48)
    cnt = [0]

    def nxt():
        k = keys[cnt[0]]
        cnt[0] += 1
        return k

    def nrm(shape, scale):
        return scale * jax.random.normal(nxt(), shape, jnp.float32)

    NA, NB, ND, NM = N_RGLRU_LAYERS, N_CONV_LAYERS, N_DENSE_LAYERS, N_MOE_LAYERS
    D, LW = D_MODEL, LRU_WIDTH
    a0 = jax.random.uniform(nxt(), (NA, LW), jnp.float32, 0.9, 0.999)
    s = a0 ** (1.0 / LRU_C)
    lam = jnp.log(s) - jnp.log1p(-s)
    return {
        'x_prompt': nrm((BATCH, SEQ, D), 1.0),
        'x_sample': nrm((DEC_BATCH, DEC_SEQ, D), 1.0),
        'p_prompt': nrm((DEPTH, BATCH, SEQ, PLE_DIM), 1.0),
        'p_sample': nrm((DEPTH, DEC_BATCH, DEC_SEQ, PLE_DIM), 1.0),
        'state_rglru_conv': nrm((NA, DEC_BATCH, LRU_CONV_W - 1, LW), 1.0),
        'state_rglru_h': nrm((NA, DEC_BATCH, LW), 0.5),
        'state_conv_module': nrm((NB, DEC_BATCH, CM_KERNEL - 1, D), 0.5),
        'rglru_w_in': nrm((NA, D, 2 * LW), D ** -0.5),
        'rglru_conv_w': nrm((NA, LRU_CONV_W, LW), LRU_CONV_W ** -0.5),
        'rglru_conv_b': nrm((NA, LW), 0.01),
        'rglru_w_a': nrm((NA, LRU_HEADS, LRU_BLOCK, LRU_BLOCK), LRU_BLOCK ** -0.5),
        'rglru_b_a': nrm((NA, LW), 0.01),
        'rglru_w_x': nrm((NA, LRU_HEADS, LRU_BLOCK, LRU_BLOCK), LRU_BLOCK ** -0.5),
        'rglru_b_x': nrm((NA, LW), 0.01),
        'rglru_lambda': lam,
        'rglru_w_out': nrm((NA, LW, D), DEEPNORM_BETA * LW ** -0.5),
        'cm_w_pw1': nrm((NB, D, 2 * D), D ** -0.5),
        'cm_b_pw1': nrm((NB, 2 * D), 0.01),
        'cm_dw_w': nrm((NB, CM_KERNEL, D), CM_KERNEL ** -0.5),
        'cm_dw_b': nrm((NB, D), 0.01),
        'cm_ln_g': 1.0 + nrm((NB, D), 0.01),
        'cm_ln_b': nrm((NB, D), 0.01),
        'cm_w_pw2': nrm((NB, D, D), DEEPNORM_BETA * D ** -0.5),
        'cm_b_pw2': nrm((NB, D), 0.01),
        'ffn_w_in': nrm((ND, D, 2 * D_FF), D ** -0.5),
        'ffn_w_out': nrm((ND, D_FF, D), DEEPNORM_BETA * D_FF ** -0.5),
        'moe_w_router': nrm((NM, D, N_EXPERTS), D ** -0.5),
        'moe_w_in': nrm((NM, N_EXPERTS, D, 2 * MOE_D_FF), D ** -0.5),
        'moe_w_out': nrm((NM, N_EXPERTS, MOE_D_FF, D), DEEPNORM_BETA * MOE_D_FF ** -0.5),
        'ln_mix_g': 1.0 + nrm((DEPTH, D), 0.01),
        'ln_mix_b': nrm((DEPTH, D), 0.01),
        'ln_ffn_g': 1.0 + nrm((DEPTH, D), 0.01),
        'ln_ffn_b': nrm((DEPTH, D), 0.01),
        'ple_w_proj': nrm((DEPTH, PLE_DIM, D), 0.5 * PLE_DIM ** -0.5),
        'ple_w_gate': nrm((DEPTH, D, D), D ** -0.5),
        'ple_b_gate': nrm((DEPTH, D), 0.01),
    }


def reference(x_prompt, x_sample, p_prompt, p_sample, state_rglru_conv, state_rglru_h, state_conv_module,
              rglru_w_in, rglru_conv_w, rglru_conv_b, rglru_w_a, rglru_b_a, rglru_w_x, rglru_b_x,
              rglru_lambda, rglru_w_out,
              cm_w_pw1, cm_b_pw1, cm_dw_w, cm_dw_b, cm_ln_g, cm_ln_b, cm_w_pw2, cm_b_pw2,
              ffn_w_in, ffn_w_out, moe_w_router, moe_w_in, moe_w_out,
              ln_mix_g, ln_mix_b, ln_ffn_g, ln_ffn_b, ple_w_proj, ple_w_gate, ple_b_gate):
    weights = (rglru_w_in, rglru_conv_w, rglru_conv_b, rglru_w_a, rglru_b_a, rglru_w_x, rglru_b_x,
               rglru_lambda, rglru_w_out,
               cm_w_pw1, cm_b_pw1, cm_dw_w, cm_dw_b, cm_ln_g, cm_ln_b, cm_w_pw2, cm_b_pw2,
               ffn_w_in, ffn_w_out, moe_w_router, moe_w_in, moe_w_out,
               ln_mix_g, ln_mix_b, ln_ffn_g, ln_ffn_b, ple_w_proj, ple_w_gate, ple_b_gate)
    bp = x_prompt.shape[0]
    dt = x_prompt.dtype
    zero_conv = jnp.zeros((N_RGLRU_LAYERS, bp, LRU_CONV_W - 1, LRU_WIDTH), dt)
    zero_h = jnp.zeros((N_RGLRU_LAYERS, bp, LRU_WIDTH), dt)
    zero_cm = jnp.zeros((N_CONV_LAYERS, bp, CM_KERNEL - 1, D_MODEL), dt)
    y_prompt, conv_p, h_p, cm_p = trunk(x_prompt, p_prompt, zero_conv, zero_h, zero_cm, *weights)
    y_sample, conv_s, h_s, cm_s = trunk(x_sample, p_sample, state_rglru_conv, state_rglru_h,
                                        state_conv_module, *weights)
    return (y_prompt, y_sample, conv_p, h_p, cm_p, conv_s, h_s, cm_s)
```

```python
import os
from contextlib import ExitStack

import numpy as np
import concourse.bass as bass
import concourse.mybir as mybir
from concourse.bass_utils import run_bass_kernel_spmd

F32 = mybir.dt.float32
BF16 = mybir.dt.bfloat16
AF = mybir.ActivationFunctionType
ALU = mybir.AluOpType

NCORES = 8
D = 2048
NCH = 16
SEQ = 2048
TT = 512
DEC = 16
PLE = 256
DFF = 5632
EFF = 2816
NE = 8
ALPHA = (2.0 * 2) ** 0.25
EPS = 1e-5
NST = 34
KB = 16
NSLOT = 2
NTMP = 8

_cv = {}
_off = 0
for _n, _w in [("convw", 64), ("convb", 16), ("ba", 16), ("bx", 16), ("lam", 16),
               ("bpw1", 32), ("dww", 496), ("dwb", 16), ("cmg", 16), ("cmb", 16), ("bpw2", 16),
               ("mixg0", 16), ("mixb0", 16), ("mixg1", 16), ("mixb1", 16),
               ("ffng0", 16), ("ffnb0", 16), ("ffng1", 16), ("ffnb1", 16),
               ("pleb0", 16), ("pleb1", 16), ("wr", 128), ("cl", 16), ("cl2", 16)]:
    _cv[_n] = (_off, _w)
    _off += _w
NV = _off


class _Eng:
    def __init__(self, name, eng, sem, self_sync):
        self.name, self.eng, self.sem, self.self_sync = name, eng, sem, self_sync
        self.count = 0
        self.waited = {}


class _Stream:
    def __init__(self, name, sem):
        self.name, self.sem, self.count = name, sem, 0


class _FW:
    def __init__(self):
        self.lastw = {}
        self.readers = {}
        self.nwait = 0
        self.ninst = 0

    def _deps(self, reads, writes):
        deps = []
        for k in reads:
            t = self.lastw.get(k)
            if t is not None:
                deps.append(t)
        for k in writes:
            t = self.lastw.get(k)
            if t is not None:
                deps.append(t)
            deps.extend(self.readers.get(k, ()))
        return deps

    def _emit_waits(self, E, deps):
        best = {}
        for (sem, val, who) in deps:
            if who == E.name and not E.self_sync:
                continue
            if E.waited.get(id(sem), 0) >= val:
                continue
            if best.get(id(sem), (None, 0))[1] < val:
                best[id(sem)] = (sem, val)
        for sem, val in best.values():
            E.eng.wait_ge(sem, val)
            E.waited[id(sem)] = val
            self.nwait += 1

    def _record(self, tok, reads, writes):
        for k in writes:
            self.lastw[k] = tok
            self.readers[k] = []
        for k in reads:
            self.readers.setdefault(k, []).append(tok)

    def op(self, E, fn, reads=(), writes=(), inc=True):
        self._emit_waits(E, self._deps(reads, writes))
        inst = fn()
        self.ninst += 1
        if inc:
            inst.then_inc(E.sem, 1)
            E.count += 1
            tok = (E.sem, E.count, E.name)
        else:
            tok = (E.sem, E.count + 1, E.name)
        self._record(tok, reads, writes)

    def dma(self, Q, S, out, in_, reads=(), writes=(), **kw):
        self._emit_waits(Q, self._deps(reads, writes))
        inst = Q.eng.dma_start(out=out, in_=in_, **kw)
        self.ninst += 1
        inst.then_inc(S.sem, 16)
        S.count += 16
        tok = (S.sem, S.count, S.name)
        self._record(tok, reads, writes)


def build_program(tile_sel=None):
    nc = bass.Bass("TRN2", target_bir_lowering=False)
    es = ExitStack()

    def din(name, shape, dt=F32):
        return nc.dram_tensor(name, list(shape), dt, kind="ExternalInput").ap()

    def dout(name, shape):
        return nc.dram_tensor(name, list(shape), F32, kind="ExternalOutput").ap()

    def dscr(name, shape):
        return nc.dram_tensor(name, list(shape), BF16, kind="Internal").ap()

    xTp = din("xTp", [2, 128, NCH, SEQ])
    xTs = din("xTs", [128, NCH, 2 * DEC])
    pTp = din("pTp", [2, 2, 128, 2, SEQ])
    pTs = din("pTs", [2, 128, 2, 2 * DEC])
    st_in = din("st_in", [2, 128, NCH, NST])
    cvec_d = din("cvec", [128, NV])
    ident_d = din("ident", [128, 128])
    selmat_d = din("selmat", [NE, NE * 128])
    wsrc = {
        "rg_in": din("rglru_w_in", [D, 2 * D]),
        "rg_a": din("rglru_w_a", [D, 256]),
        "rg_x": din("rglru_w_x", [D, 256]),
        "rg_out": din("rglru_w_out", [D, D]),
        "pw1": din("cm_w_pw1", [D, 2 * D]),
        "pw2": din("cm_w_pw2", [D, D]),
        "ffn_in": din("ffn_w_in", [D, 2 * DFF]),
        "ffn_out": din("ffn_w_out", [DFF, D]),
        "moe_in": din("moe_w_in", [NE * D, 2 * EFF]),
        "moe_out": din("moe_w_out", [NE * EFF, D]),
        "pproj0": din("ple_w_proj0", [PLE, D]),
        "pproj1": din("ple_w_proj1", [PLE, D]),
        "pgate0": din("ple_w_gate0", [D, D]),
        "pgate1": din("ple_w_gate1", [D, D]),
    }
    WSPEC = {
        "rg_in": ("plain", 1, D, 8), "rg_out": ("plain", 1, D, 4), "pw2": ("plain", 1, D, 4),
        "pgate0": ("plain", 1, D, 4), "pgate1": ("plain", 1, D, 4),
        "pproj0": ("plain", 1, PLE, 4), "pproj1": ("plain", 1, PLE, 4),
        "ffn_out": ("plain", 2, EFF, 4), "moe_out": ("plain", NE, EFF, 4),
        "pw1": ("paired", 1, D, 8), "ffn_in": ("paired", 1, D, 22), "moe_in": ("paired", NE, D, 11),
    }
    wbf = {k: dscr("bf_" + k, [S * Gs, 128, rows // 128, 512]) for k, (kind, S, rows, Gs) in WSPEC.items()}
    wbf["rg_ax"] = dscr("bf_rg_ax", [1, 128, NCH, 512])
    yTp = dout("yTp", [2, 128, NCH, SEQ])
    yTs = dout("yTs", [128, NCH, 2 * DEC])
    st_out_p = dout("st_out_p", [2, 128, NCH, NST])
    st_out_s = dout("st_out_s", [2, 128, NCH, NST])

    def sb(name, shape, dt=F32):
        return es.enter_context(nc.sbuf_tensor(name, list(shape), dt))

    X = [sb("X0", [128, NCH, TT]), sb("X1", [128, NCH, TT])]
    xb = sb("xb", [128, NCH, TT], BF16)
    G = sb("G", [128, NCH, TT], BF16)
    H = sb("H", [128, 22, TT], BF16)
    Wsl = [sb(f"W{i}", [128, KB, 512], BF16) for i in range(NSLOT)]
    tmps = [sb(f"tmp{i}", [128, TT]) for i in range(NTMP)]
    cbufs = [sb(f"cb{i}", [128, 544]) for i in range(2)]
    pland = [sb("pl0", [128, 2, TT])] * 2
    gbuf = [sb(f"gbuf{i}", [128, TT]) for i in range(2)]
    lnm = sb("lnm", [128, TT])
    lnv = sb("lnv", [128, TT])
    lns1 = sb("lns1", [128, TT])
    lns2 = sb("lns2", [128, TT])
    pbf = [sb(f"pb{i}", [128, 2, TT], BF16) for i in range(2)]
    ST = [sb(f"ST{i}", [128, NCH, NST]) for i in range(2)]
    cvec = sb("cvec_sb", [128, NV])
    ident = sb("ident_sb", [128, 128])
    onesD = sb("onesD", [128, 128])
    selmat = sb("selmat_sb", [NE, NE * 128])
    rt = sb("rt", [128, 4, NE])
    rg = sb("rgates", [128, 4, NE])
    rs = sb("rsmall", [128, 4, 8])
    rt2 = sb("rt2", [128, 4, NE])
    gT = sb("gT", [NE, TT])
    banks = [es.enter_context(nc.psum_tensor(f"ps{i}", [128, 512], F32)) for i in range(8)]

    def sem(name):
        return es.enter_context(nc.semaphore(name))

    PE = _Eng("pe", nc.tensor, sem("s_pe"), False)
    ACT = _Eng("act", nc.scalar, sem("s_act"), True)
    DVE = _Eng("dve", nc.vector, sem("s_dve"), True)
    POOL = _Eng("pool", nc.gpsimd, sem("s_pool"), True)
    SP = _Eng("sp", nc.sync, sem("s_sp"), True)
    s_w = [_Stream(f"w{i}", sem(f"s_w{i}")) for i in range(NSLOT)]
    s_x = _Stream("x", sem("s_x"))
    s_p = [_Stream(f"p{i}", sem(f"s_p{i}")) for i in range(2)]
    s_st = _Stream("st", sem("s_st"))
    s_y = _Stream("y", sem("s_y"))
    s_so = _Stream("so", sem("s_so"))
    s_c = _Stream("c", sem("s_c"))
    s_castA = _Stream("castA", sem("s_castA"))
    s_castB = _Stream("castB", sem("s_castB"))
    s_castC = _Stream("castC", sem("s_castC"))
    s_castD = _Stream("castD", sem("s_castD"))

    fw = _FW()
    state = {"bank": 0, "tmp": 0, "slot": 0, "cb": 0, "use_pool": False}

    def cv(name, c=None, k=None):
        o, w = _cv[name]
        if k is not None:
            return cvec[:, o + k:o + k + 1]
        if c is not None:
            return cvec[:, o + c:o + c + 1]
        return cvec[:, o:o + w]

    def next_banks(n):
        b0 = state["bank"]
        if b0 + n > 8:
            b0 = 0
        state["bank"] = (b0 + n) % 8
        return list(range(b0, b0 + n))

    def next_tmp():
        i = state["tmp"]
        state["tmp"] = (i + 1) % NTMP
        return i

    def next_cb():
        i = state["cb"]
        state["cb"] = (i + 1) % 2
        return i

    fw.dma(SP, s_c, cvec[:], cvec_d, writes=[("cvec",)])
    fw.dma(SP, s_c, ident[:], ident_d, writes=[("ident",)])
    fw.dma(SP, s_c, selmat[:], selmat_d, writes=[("selmat",)])
    for _k in (("cvec",), ("ident",), ("selmat",)):
        fw.lastw[_k] = (s_c.sem, s_c.count, "c")
    fw.op(DVE, lambda: nc.vector.memset(onesD[:], 1.0 / D), writes=[("onesD",)])
    o_lam, _ = _cv["lam"]
    o_cl, _ = _cv["cl"]
    o_cl2, _ = _cv["cl2"]
    fw.op(ACT, lambda: nc.scalar.activation(out=cvec[:, o_cl:o_cl + 16], in_=cvec[:, o_lam:o_lam + 16],
                                            func=AF.Exp, scale=-1.0), reads=[("cvec",)], writes=[("cl",)])
    fw.op(ACT, lambda: nc.scalar.activation(out=cvec[:, o_cl:o_cl + 16], in_=cvec[:, o_cl:o_cl + 16],
                                            func=AF.Ln, bias=1.0, scale=1.0), reads=[("cl",)], writes=[("cl",)])
    fw.op(ACT, lambda: nc.scalar.mul(out=cvec[:, o_cl2:o_cl2 + 16], in_=cvec[:, o_cl:o_cl + 16], mul=-16.0),
          reads=[("cl",)], writes=[("cl2",)])
    fw.op(ACT, lambda: nc.scalar.mul(out=cvec[:, o_cl:o_cl + 16], in_=cvec[:, o_cl:o_cl + 16], mul=-8.0),
          reads=[("cl",), ("cl2",)], writes=[("cl",)])
    CONST_KEYS = [("cvec",), ("cl",), ("cl2",)]

    tiles = []
    for b in range(2):
        for j in range(SEQ // TT):
            tiles.append(dict(kind="p", T=TT, segs=[(0, 0, TT)], b=b, t0=j * TT,
                              first=(j == 0), last=(j == SEQ // TT - 1)))
    tiles.append(dict(kind="s", T=2 * DEC, segs=[(0, 0, DEC), (1, DEC, DEC)], first=True, last=True))
    if tile_sel is not None:
        tiles = [tiles[i] for i in tile_sel]

    def x_src(t):
        if t["kind"] == "p":
            return xTp[t["b"], :, :, t["t0"]:t["t0"] + TT]
        return xTs

    def y_dst(t):
        if t["kind"] == "p":
            return yTp[t["b"], :, :, t["t0"]:t["t0"] + TT]
        return yTs

    def p_src(t, layer):
        if t["kind"] == "p":
            return pTp[layer, t["b"], :, :, t["t0"]:t["t0"] + TT]
        return pTs[layer]

    def xkeys(buf_i):
        return [("X", buf_i, c) for c in range(NCH)]

    t0 = tiles[0]
    fw.dma(SP, s_x, X[0][:, :, :t0["T"]], x_src(t0), writes=xkeys(0))

    cast_streams = [s_castA, s_castB, s_castC, s_castD]
    cast_toks = {}
    ncast = [0]

    def cast_dma(dv, sv):
        S = cast_streams[ncast[0] % len(cast_streams)]
        ncast[0] += 1
        if S.count:
            fw._emit_waits(POOL, [(S.sem, S.count, S.name)])
        inst = nc.gpsimd.dma_start(out=dv, in_=sv)
        inst.then_inc(S.sem, 16)
        S.count += 16
        fw.ninst += 1

    def cast_group(name, idx):
        kind, S, rows, Gs = WSPEC[name]
        src = wsrc[name]
        N = src.shape[1]
        sp, g = divmod(idx, Gs)
        r0 = sp * rows
        if kind == "plain":
            sv = src[r0:r0 + rows, g * 512:(g + 1) * 512].rearrange("(k p) n -> p k n", p=128)
            cast_dma(wbf[name][idx], sv)
        else:
            for hsel in range(2):
                c0 = hsel * (N // 2) + g * 256
                sv = src[r0:r0 + rows, c0:c0 + 256].rearrange("(k p) n -> p k n", p=128)
                cast_dma(wbf[name][idx][:, :, hsel * 256:(hsel + 1) * 256], sv)
        cast_toks[(name, idx)] = [(St.sem, St.count, St.name) for St in cast_streams if St.count]

    def cast_all(name, idxs=None):
        kind, S, rows, Gs = WSPEC[name]
        for idx in (range(S * Gs) if idxs is None else idxs):
            cast_group(name, idx)

    cast_all("rg_in", [4, 5, 6, 7, 0, 1, 2, 3])
    for hsel, nm in enumerate(("rg_a", "rg_x")):
        cast_dma(wbf["rg_ax"][0][:, :, hsel * 256:(hsel + 1) * 256],
                 wsrc[nm].rearrange("(k p) n -> p k n", p=128))
    cast_toks[("rg_ax", 0)] = [(St.sem, St.count, St.name) for St in cast_streams if St.count]
    cast_all("rg_out")
    for hf in range(2):
        cast_all("ffn_in", range(hf * 11, hf * 11 + 11))
        cast_all("ffn_out", range(hf * 4, hf * 4 + 4))
    for g in range(4):
        cast_group("pgate0", g)
        cast_group("pproj0", g)
    cast_all("pw1")
    cast_all("pw2")
    for e in range(NE):
        cast_all("moe_in", range(e * 11, e * 11 + 11))
        cast_all("moe_out", range(e * 4, e * 4 + 4))
    for g in range(4):
        cast_group("pgate1", g)
        cast_group("pproj1", g)

    after_wload = []

    def wload(name, idx, kc0, nk):
        si = state["slot"]
        state["slot"] = (si + 1) % NSLOT
        key = ("W", si)
        deps = fw._deps((), [key]) + cast_toks[(name, idx)]
        fw._emit_waits(SP, deps)
        inst = nc.sync.dma_start(out=Wsl[si][:, 0:nk, :], in_=wbf[name][idx, :, kc0:kc0 + nk, :])
        inst.then_inc(s_w[si].sem, 16)
        s_w[si].count += 16
        fw.ninst += 1
        fw._record((s_w[si].sem, s_w[si].count, s_w[si].name), (), [key])
        while after_wload:
            after_wload.pop(0)()
        return si

    def stream_group(name, T, nkc, idx, rhs_fn, rhs_keys_fn, evac_fn):
        noc = 4
        bks = next_banks(noc)
        nblk = -(-nkc // KB)
        bs = -(-nkc // nblk)
        for kb0 in range(0, nkc, bs):
            nk = min(bs, nkc - kb0)
            si = wload(name, idx, kb0, nk)
            for oi in range(noc):
                for kk in range(nk):
                    kc = kb0 + kk
                    last = (kk == nk - 1)
                    fw.op(PE, (lambda oi=oi, kk=kk, kc=kc, si=si:
                               nc.tensor.matmul(banks[bks[oi]][:, :T], lhsT=Wsl[si][:, kk, oi * 128:(oi + 1) * 128],
                                                rhs=rhs_fn(kc), start=(kc == 0), stop=(kc == nkc - 1))),
                          reads=[("W", si)] + rhs_keys_fn(kc), writes=[("B", bks[oi])], inc=last)
        evac_fn(bks)

    def xb_rhs(T):
        return (lambda kc: xb[:, kc, :T]), (lambda kc: [("xb", kc)])

    S1K, S2K = ("lns1",), ("lns2",)

    def ln_acc(src, srckey, c, T, first):
        if first:
            fw.op(DVE, lambda: nc.vector.tensor_copy(out=lns1[:, :T], in_=src[:, c, :T]),
                  reads=[srckey(c)], writes=[S1K])
            fw.op(ACT, lambda: nc.scalar.activation(out=lns2[:, :T], in_=src[:, c, :T], func=AF.Square),
                  reads=[srckey(c)], writes=[S2K])
        else:
            fw.op(DVE, lambda: nc.vector.tensor_tensor(out=lns1[:, :T], in0=lns1[:, :T], in1=src[:, c, :T], op=ALU.add),
                  reads=[srckey(c), S1K], writes=[S1K])
            ti = next_tmp()
            fw.op(ACT, lambda: nc.scalar.activation(out=tmps[ti][:, :T], in_=src[:, c, :T], func=AF.Square),
                  reads=[srckey(c)], writes=[("tmp", ti)])
            fw.op(DVE, lambda: nc.vector.tensor_tensor(out=lns2[:, :T], in0=lns2[:, :T], in1=tmps[ti][:, :T], op=ALU.add),
                  reads=[("tmp", ti), S2K], writes=[S2K])

    def ln_stats(src, srckey, T):
        bA, bB = next_banks(2)
        fw.op(PE, lambda: nc.tensor.matmul(banks[bA][:, :T], lhsT=onesD[:], rhs=lns1[:, :T], start=True, stop=True),
              reads=[("onesD",), S1K], writes=[("B", bA)])
        fw.op(PE, lambda: nc.tensor.matmul(banks[bB][:, :T], lhsT=onesD[:], rhs=lns2[:, :T], start=True, stop=True),
              reads=[("onesD",), S2K], writes=[("B", bB)])
        LM, LV = ("lnm",), ("lnv",)
        fw.op(ACT, lambda: nc.scalar.copy(out=lnm[:, :T], in_=banks[bA][:, :T]),
              reads=[("B", bA)], writes=[LM])
        fw.op(DVE, lambda: nc.vector.tensor_tensor(out=lnv[:, :T], in0=lnm[:, :T], in1=lnm[:, :T],
                                                   op=ALU.mult), reads=[LM], writes=[LV])
        fw.op(DVE, lambda: nc.vector.tensor_tensor(out=lnv[:, :T], in0=banks[bB][:, :T], in1=lnv[:, :T],
                                                   op=ALU.subtract), reads=[("B", bB), LV], writes=[LV])
        fw.op(DVE, lambda: nc.vector.tensor_scalar_max(out=lnv[:, :T], in0=lnv[:, :T], scalar1=0.0),
              reads=[LV], writes=[LV])
        fw.op(ACT, lambda: nc.scalar.activation(out=lnv[:, :T], in_=lnv[:, :T], func=AF.Sqrt,
                                                bias=EPS, scale=1.0), reads=[LV], writes=[LV])
        fw.op(DVE, lambda: nc.vector.reciprocal(out=lnv[:, :T], in_=lnv[:, :T]),
              reads=[LV], writes=[LV])
        fw.op(DVE, lambda: nc.vector.scalar_tensor_tensor(out=lnm[:, :T], in0=lnm[:, :T], scalar=-1.0,
                                                          in1=lnv[:, :T], op0=ALU.mult, op1=ALU.mult),
              reads=[LM, LV], writes=[LM])
        return LM, LV

    def ln_apply(src, srckey, T, gname, bname, out_specs):
        LM, LV = ln_stats(src, srckey, T)
        for c0 in range(0, NCH, 2):
            tis = [next_tmp(), next_tmp()]
            for q in range(2):
                c, ti = c0 + q, tis[q]
                fw.op(DVE, lambda c=c, ti=ti: nc.vector.tensor_tensor(out=tmps[ti][:, :T], in0=src[:, c, :T],
                                                                      in1=lnv[:, :T], op=ALU.mult),
                      reads=[srckey(c), LV], writes=[("tmp", ti)])
            for q in range(2):
                ti = tis[q]
                fw.op(DVE, lambda ti=ti: nc.vector.tensor_tensor(out=tmps[ti][:, :T], in0=tmps[ti][:, :T],
                                                                 in1=lnm[:, :T], op=ALU.add),
                      reads=[("tmp", ti), LM], writes=[("tmp", ti)])
            for q in range(2):
                c, ti = c0 + q, tis[q]
                for oi_, (func, dst_fn, key_fn) in enumerate(out_specs):
                    if oi_ == 1 and state["use_pool"] and func == AF.Identity:
                        fw.op(POOL, lambda c=c, ti=ti, dst_fn=dst_fn:
                              nc.gpsimd.tensor_scalar(out=dst_fn(c), in0=tmps[ti][:, :T], scalar1=cv(gname, c),
                                                      scalar2=cv(bname, c), op0=ALU.mult, op1=ALU.add),
                              reads=[("tmp", ti)] + CONST_KEYS, writes=[key_fn(c)])
                        continue
                    fw.op(ACT, lambda c=c, ti=ti, func=func, dst_fn=dst_fn:
                          nc.scalar.activation(out=dst_fn(c), in_=tmps[ti][:, :T], func=func,
                                               bias=cv(bname, c), scale=cv(gname, c)),
                          reads=[("tmp", ti)] + CONST_KEYS, writes=[key_fn(c)])

    def expert(T, xi, in_name, in_base, out_name, out_base, gate_e, first, ln_final=False):
        xk = lambda c: ("X", xi, c)
        rhs, rkeys = xb_rhs(T)
        for j in range(EFF // 256):
            def evac(bks, j=j):
                for oi in range(2):
                    bk, bu = bks[oi], bks[oi + 2]
                    ti = next_tmp()
                    fw.op(ACT, lambda: nc.scalar.activation(out=tmps[ti][:, :T], in_=banks[bk][:, :T], func=AF.Silu),
                          reads=[("B", bk)], writes=[("tmp", ti)])
                    fw.op(DVE, lambda: nc.vector.tensor_tensor(out=H[:, 2 * j + oi, :T], in0=banks[bu][:, :T],
                                                               in1=tmps[ti][:, :T], op=ALU.mult),
                          reads=[("B", bu), ("tmp", ti)], writes=[("H", 2 * j + oi)])
            stream_group(in_name, T, NCH, in_base + j, rhs, rkeys, evac)
        gi = None
        if gate_e is not None:
            gi = gate_e % 2
            bg = next_banks(1)[0]
            fw.op(PE, lambda: nc.tensor.matmul(banks[bg][:, :T], lhsT=selmat[:, gate_e * 128:(gate_e + 1) * 128],
                                               rhs=gT[:, :T], start=True, stop=True),
                  reads=[("selmat",), ("gT",)], writes=[("B", bg)])
            fw.op(ACT, lambda: nc.scalar.copy(out=gbuf[gi][:, :T], in_=banks[bg][:, :T]),
                  reads=[("B", bg)], writes=[("gbuf", gi)])
        hrhs = lambda kc: H[:, kc, :T]
        hkeys = lambda kc: [("H", kc)]
        for g in range(4):
            def evac(bks, g=g):
                for oi, bk in enumerate(bks):
                    oc = g * 4 + oi
                    src_ap = banks[bk][:, :T]
                    rk = [("B", bk)]
                    if gate_e is not None:
                        ti = next_tmp()
                        fw.op(DVE, lambda: nc.vector.tensor_tensor(out=tmps[ti][:, :T], in0=banks[bk][:, :T],
                                                                   in1=gbuf[gi][:, :T], op=ALU.mult),
                              reads=[("B", bk), ("gbuf", gi)], writes=[("tmp", ti)])
                        src_ap = tmps[ti][:, :T]
                        rk = [("tmp", ti)]
                    if first:
                        fw.op(DVE, lambda: nc.vector.scalar_tensor_tensor(out=X[xi][:, oc, :T], in0=X[xi][:, oc, :T],
                                                                          scalar=ALPHA, in1=src_ap, op0=ALU.mult, op1=ALU.add),
                              reads=rk + [xk(oc)], writes=[xk(oc)])
                    else:
                        fw.op(DVE, lambda: nc.vector.tensor_tensor(out=X[xi][:, oc, :T], in0=X[xi][:, oc, :T],
                                                                   in1=src_ap, op=ALU.add),
                              reads=rk + [xk(oc)], writes=[xk(oc)])
                    if ln_final:
                        ln_acc(X[xi], xk, oc, T, first=(oc == 0))
            stream_group(out_name, T, EFF // 128, out_base + g, hrhs, hkeys, evac)

    def ple(T, xi, layer, need_xb):
        xk = lambda c: ("X", xi, c)
        rhs, rkeys = xb_rhs(T)
        prhs = lambda kc: pbf[layer][:, kc, :T]
        pkeys = lambda kc: [("pbf", layer)]
        for g in range(4):
            sig = {}

            def evac_gate(bks, g=g, sig=sig):
                for oi, bk in enumerate(bks):
                    oc = g * 4 + oi
                    ti = next_tmp()
                    fw.op(ACT, lambda: nc.scalar.activation(out=tmps[ti][:, :T], in_=banks[bk][:, :T], func=AF.Sigmoid,
                                                            bias=cv(f"pleb{layer}", oc), scale=1.0),
                          reads=[("B", bk)] + CONST_KEYS, writes=[("tmp", ti)])
                    sig[oi] = ti

            def evac_proj(bks, g=g, sig=sig):
                for oi, bk in enumerate(bks):
                    oc = g * 4 + oi
                    ti = sig[oi]
                    fw.op(DVE, lambda: nc.vector.tensor_tensor(out=tmps[ti][:, :T], in0=banks[bk][:, :T],
                                                               in1=tmps[ti][:, :T], op=ALU.mult),
                          reads=[("B", bk), ("tmp", ti)], writes=[("tmp", ti)])
                    fw.op(DVE, lambda: nc.vector.tensor_tensor(out=X[xi][:, oc, :T], in0=X[xi][:, oc, :T],
                                                               in1=tmps[ti][:, :T], op=ALU.add),
                          reads=[("tmp", ti), xk(oc)], writes=[xk(oc)])
            stream_group(f"pgate{layer}", T, NCH, g, rhs, rkeys, evac_gate)
            stream_group(f"pproj{layer}", T, 2, g, prhs, pkeys, evac_proj)
        if need_xb:
            for oc in range(NCH):
                if oc % 2:
                    fw.op(ACT, lambda: nc.scalar.copy(out=xb[:, oc, :T], in_=X[xi][:, oc, :T]),
                          reads=[xk(oc)], writes=[("xb", oc)])
                else:
                    fw.op(DVE, lambda: nc.vector.tensor_copy(out=xb[:, oc, :T], in_=X[xi][:, oc, :T]),
                          reads=[xk(oc)], writes=[("xb", oc)])

    for ti_, t in enumerate(tiles):
        T = t["T"]
        xi = ti_ % 2
        ci = 1 - xi
        Xc = X[xi]
        C = X[ci]
        xk = lambda c, xi=xi: ("X", xi, c)
        ck = lambda c, ci=ci: ("X", ci, c)
        segs = t["segs"]
        state["use_pool"] = (ti_ >= 1)

        if t["first"]:
            if t["kind"] == "p":
                fw.op(DVE, lambda: nc.vector.memset(ST[0][:], 0.0), writes=[("ST", 0)])
            else:
                for (si_, _, _) in segs:
                    fw.dma(SP, s_st, ST[si_][:], st_in[si_], writes=[("ST", si_)])
                for (si_, _, _) in segs:
                    fw.lastw[("ST", si_)] = (s_st.sem, s_st.count, "st")
        for layer in range(2):
            fw.dma(SP, s_p[layer], pland[layer][:, :, :T], p_src(t, layer), writes=[("pland",)])
            fw.op(DVE, lambda layer=layer: nc.vector.tensor_copy(out=pbf[layer][:, :, :T], in_=pland[layer][:, :, :T]),
                  reads=[("pland",)], writes=[("pbf", layer)])
        for c in range(NCH):
            E = ACT if c % 2 else DVE
            if E is ACT:
                fw.op(ACT, lambda c=c: nc.scalar.copy(out=xb[:, c, :T], in_=Xc[:, c, :T]),
                      reads=[xk(c)], writes=[("xb", c)])
            else:
                fw.op(DVE, lambda c=c: nc.vector.tensor_copy(out=xb[:, c, :T], in_=Xc[:, c, :T]),
                      reads=[xk(c)], writes=[("xb", c)])

        rhs, rkeys = xb_rhs(T)
        for g in range(4):
            def evac_rec(bks, g=g):
              for oi, bk in enumerate(bks):
                c = g * 4 + oi
                for (si_, col0, ln) in segs:
                    cbi = next_cb()
                    cb = cbufs[cbi]
                    fw.op(DVE, lambda: nc.vector.tensor_copy(out=cb[:, 0:3], in_=ST[si_][:, c, 0:3]),
                          reads=[("ST", si_)], writes=[("cb", cbi)])
                    fw.op(ACT, lambda: nc.scalar.copy(out=cb[:, 3:3 + ln], in_=banks[bk][:, col0:col0 + ln]),
                          reads=[("B", bk), ("cb", cbi)], writes=[("cb", cbi)])
                    xc = C[:, c, col0:col0 + ln]
                    ta = next_tmp()
                    a1 = tmps[ta][:, :ln]
                    fw.op(DVE, lambda: nc.vector.tensor_scalar(out=xc, in0=cb[:, 0:ln], scalar1=cv("convw", k=c * 4 + 0),
                                                               scalar2=cv("convb", c), op0=ALU.mult, op1=ALU.add),
                          reads=[("cb", cbi)] + CONST_KEYS, writes=[ck(c)])
                    fw.op(DVE, lambda: nc.vector.tensor_scalar(out=a1, in0=cb[:, 1:1 + ln], scalar1=cv("convw", k=c * 4 + 1),
                                                               scalar2=None, op0=ALU.mult),
                          reads=[("cb", cbi)] + CONST_KEYS, writes=[("tmp", ta)])
                    fw.op(DVE, lambda: nc.vector.scalar_tensor_tensor(out=xc, in0=cb[:, 2:2 + ln],
                                                                      scalar=cv("convw", k=c * 4 + 2), in1=xc,
                                                                      op0=ALU.mult, op1=ALU.add),
                          reads=[("cb", cbi), ck(c)] + CONST_KEYS, writes=[ck(c)])
                    fw.op(DVE, lambda: nc.vector.scalar_tensor_tensor(out=a1, in0=cb[:, 3:3 + ln],
                                                                      scalar=cv("convw", k=c * 4 + 3), in1=a1,
                                                                      op0=ALU.mult, op1=ALU.add),
                          reads=[("cb", cbi), ("tmp", ta)] + CONST_KEYS, writes=[("tmp", ta)])
                    fw.op(DVE, lambda: nc.vector.tensor_tensor(out=xc, in0=xc, in1=a1, op=ALU.add),
                          reads=[ck(c), ("tmp", ta)], writes=[ck(c)])
                    fw.op(ACT, lambda: nc.scalar.copy(out=ST[si_][:, c, 0:3], in_=cb[:, ln:ln + 3]),
                          reads=[("cb", cbi), ("ST", si_)], writes=[("ST", si_)])
                fw.op(ACT, lambda: nc.scalar.copy(out=H[:, c, :T], in_=C[:, c, :T]),
                      reads=[ck(c)], writes=[("H", c)])
            stream_group("rg_in", T, NCH, 4 + g, rhs, rkeys, evac_rec)
        for g in range(4):
            def evac_gate(bks, g=g):
                for oi, bk in enumerate(bks):
                    c = g * 4 + oi
                    fw.op(ACT, lambda: nc.scalar.activation(out=G[:, c, :T], in_=banks[bk][:, :T], func=AF.Gelu),
                          reads=[("B", bk)], writes=[("G", c)])
            stream_group("rg_in", T, NCH, g, rhs, rkeys, evac_gate)

        for kb0 in range(0, NCH, KB):
            si_w = wload("rg_ax", 0, kb0, KB)
            for hh in range(KB // 2):
                h = kb0 // 2 + hh
                for j in range(2):
                    oc = 2 * h + j
                    bA, bX = next_banks(2)
                    for (bk, coff) in ((bA, 0), (bX, 256)):
                        for i in range(2):
                            kk = 2 * hh + i
                            fw.op(PE, lambda bk=bk, coff=coff, kk=kk, i=i, j=j, h=h:
                                  nc.tensor.matmul(banks[bk][:, :T], lhsT=Wsl[si_w][:, kk, coff + j * 128:coff + (j + 1) * 128],
                                                   rhs=H[:, 2 * h + i, :T], start=(i == 0), stop=(i == 1)),
                                  reads=[("W", si_w), ("H", 2 * h + i)], writes=[("B", bk)], inc=(i == 1))
                    t1, t2, t3 = next_tmp(), next_tmp(), next_tmp()
                    fw.op(ACT, lambda: nc.scalar.activation(out=tmps[t1][:, :T], in_=banks[bA][:, :T], func=AF.Sigmoid,
                                                            bias=cv("ba", oc), scale=1.0),
                          reads=[("B", bA)] + CONST_KEYS, writes=[("tmp", t1)])
                    fw.op(ACT, lambda: nc.scalar.activation(out=tmps[t2][:, :T], in_=banks[bX][:, :T], func=AF.Sigmoid,
                                                            bias=cv("bx", oc), scale=1.0),
                          reads=[("B", bX)] + CONST_KEYS, writes=[("tmp", t2)])
                    fw.op(ACT, lambda: nc.scalar.activation(out=tmps[t3][:, :T], in_=tmps[t1][:, :T], func=AF.Exp,
                                                            scale=cv("cl2", oc)),
                          reads=[("tmp", t1)] + CONST_KEYS, writes=[("tmp", t3)])
                    fw.op(ACT, lambda: nc.scalar.activation(out=tmps[t1][:, :T], in_=tmps[t1][:, :T], func=AF.Exp,
                                                            scale=cv("cl", oc)),
                          reads=[("tmp", t1)] + CONST_KEYS, writes=[("tmp", t1)])
                    fw.op(ACT, lambda: nc.scalar.activation(out=tmps[t3][:, :T], in_=tmps[t3][:, :T], func=AF.Relu,
                                                            bias=1.0, scale=-1.0),
                          reads=[("tmp", t3)], writes=[("tmp", t3)])
                    fw.op(ACT, lambda: nc.scalar.activation(out=tmps[t3][:, :T], in_=tmps[t3][:, :T], func=AF.Sqrt),
                          reads=[("tmp", t3)], writes=[("tmp", t3)])
                    fw.op(DVE, lambda: nc.vector.tensor_tensor(out=tmps[t2][:, :T], in0=tmps[t2][:, :T],
                                                               in1=C[:, oc, :T], op=ALU.mult),
                          reads=[("tmp", t2), ck(oc)], writes=[("tmp", t2)])
                    fw.op(DVE, lambda: nc.vector.tensor_tensor(out=tmps[t2][:, :T], in0=tmps[t2][:, :T],
                                                               in1=tmps[t3][:, :T], op=ALU.mult),
                          reads=[("tmp", t2), ("tmp", t3)], writes=[("tmp", t2)])
                    for (si_, col0, ln) in segs:
                        fw.op(DVE, lambda si_=si_, col0=col0, ln=ln:
                              nc.vector.tensor_tensor_scan(out=tmps[t3][:, col0:col0 + ln], data0=tmps[t1][:, col0:col0 + ln],
                                                           data1=tmps[t2][:, col0:col0 + ln], initial=ST[si_][:, oc, 3:4],
                                                           op0=ALU.mult, op1=ALU.add),
                              reads=[("tmp", t1), ("tmp", t2), ("tmp", t3), ("ST", si_)], writes=[("tmp", t3)])
                        fw.op(ACT, lambda si_=si_, col0=col0, ln=ln:
                              nc.scalar.copy(out=ST[si_][:, oc, 3:4], in_=tmps[t3][:, col0 + ln - 1:col0 + ln]),
                              reads=[("tmp", t3), ("ST", si_)], writes=[("ST", si_)])
                    fw.op(DVE, lambda: nc.vector.tensor_tensor(out=xb[:, oc, :T], in0=tmps[t3][:, :T], in1=G[:, oc, :T],
                                                               op=ALU.mult),
                          reads=[("tmp", t3), ("G", oc)], writes=[("xb", oc)])
        for g in range(4):
            def evac_o(bks, g=g):
                for oi, bk in enumerate(bks):
                    oc = g * 4 + oi
                    fw.op(DVE, lambda: nc.vector.scalar_tensor_tensor(out=Xc[:, oc, :T], in0=Xc[:, oc, :T], scalar=ALPHA,
                                                                      in1=banks[bk][:, :T], op0=ALU.mult, op1=ALU.add),
                          reads=[("B", bk), xk(oc)], writes=[xk(oc)])
                    ln_acc(Xc, xk, oc, T, first=(oc == 0))
            stream_group("rg_out", T, NCH, g, rhs, rkeys, evac_o)
        ln_apply(Xc, xk, T, "mixg0", "mixb0",
                 [(AF.Identity, lambda c: Xc[:, c, :T], xk), (AF.Identity, lambda c: xb[:, c, :T], lambda c: ("xb", c))])

        for hf in range(2):
            expert(T, xi, "ffn_in", hf * 11, "ffn_out", hf * 4, None, first=(hf == 0), ln_final=(hf == 1))
        ln_apply(Xc, xk, T, "ffng0", "ffnb0",
                 [(AF.Identity, lambda c: Xc[:, c, :T], xk), (AF.Identity, lambda c: xb[:, c, :T], lambda c: ("xb", c))])
        ple(T, xi, 0, need_xb=True)

        rhs, rkeys = xb_rhs(T)
        for j in range(8):
            def evac_pw1(lst, j=j):
                for q in range(2):
                    c = 2 * j + q
                    bval, bgate = lst[q], lst[2 + q]
                    ti = next_tmp()
                    fw.op(ACT, lambda: nc.scalar.activation(out=tmps[ti][:, :T], in_=banks[bgate][:, :T], func=AF.Sigmoid,
                                                            bias=cv("bpw1", 16 + c), scale=1.0),
                          reads=[("B", bgate)] + CONST_KEYS, writes=[("tmp", ti)])
                    for (si_, col0, ln) in segs:
                        cbi = next_cb()
                        cb = cbufs[cbi]
                        fw.op(ACT, lambda: nc.scalar.copy(out=cb[:, 0:30], in_=ST[si_][:, c, 4:34]),
                              reads=[("ST", si_)], writes=[("cb", cbi)])
                        fw.op(DVE, lambda: nc.vector.scalar_tensor_tensor(out=cb[:, 30:30 + ln], in0=banks[bval][:, col0:col0 + ln],
                                                                          scalar=cv("bpw1", c), in1=tmps[ti][:, col0:col0 + ln],
                                                                          op0=ALU.add, op1=ALU.mult),
                              reads=[("B", bval), ("tmp", ti), ("cb", cbi)] + CONST_KEYS, writes=[("cb", cbi)])
                        y = C[:, c, col0:col0 + ln]
                        ta, tb = next_tmp(), next_tmp()
                        accs = [(y, ck(c)), (tmps[ta][:, :ln], ("tmp", ta)), (tmps[tb][:, :ln], ("tmp", tb))]
                        for k in range(31):
                            acc, akey = accs[k % 3]
                            wk = cv("dww", k=c * 31 + k)
                            if k == 0:
                                fw.op(DVE, lambda: nc.vector.tensor_scalar(out=acc, in0=cb[:, 0:ln], scalar1=wk,
                                                                           scalar2=cv("dwb", c), op0=ALU.mult, op1=ALU.add),
                                      reads=[("cb", cbi)] + CONST_KEYS, writes=[akey])
                            elif k < 3:
                                fw.op(DVE, lambda: nc.vector.tensor_scalar(out=acc, in0=cb[:, k:k + ln], scalar1=wk,
                                                                           scalar2=None, op0=ALU.mult),
                                      reads=[("cb", cbi)] + CONST_KEYS, writes=[akey])
                            else:
                                fw.op(DVE, lambda: nc.vector.scalar_tensor_tensor(out=acc, in0=cb[:, k:k + ln], scalar=wk,
                                                                                  in1=acc, op0=ALU.mult, op1=ALU.add),
                                      reads=[("cb", cbi), akey] + CONST_KEYS, writes=[akey])
                        for (acc, akey) in accs[1:]:
                            fw.op(DVE, lambda: nc.vector.tensor_tensor(out=y, in0=y, in1=acc, op=ALU.add),
                                  reads=[ck(c), akey], writes=[ck(c)])
                        fw.op(ACT, lambda: nc.scalar.copy(out=ST[si_][:, c, 4:34], in_=cb[:, ln:ln + 30]),
                              reads=[("cb", cbi), ("ST", si_)], writes=[("ST", si_)])
                    ln_acc(C, ck, c, T, first=(c == 0))
            stream_group("pw1", T, NCH, j, rhs, rkeys, evac_pw1)
        ln_apply(C, ck, T, "cmg", "cmb", [(AF.Silu, lambda c: G[:, c, :T], lambda c: ("G", c))])
        grhs = lambda kc: G[:, kc, :T]
        gkeys = lambda kc: [("G", kc)]
        for g in range(4):
            def evac_pw2(bks, g=g):
              for oi, bk in enumerate(bks):
                oc = g * 4 + oi
                ti = next_tmp()
                fw.op(ACT, lambda: nc.scalar.activation(out=tmps[ti][:, :T], in_=banks[bk][:, :T], func=AF.Identity,
                                                        bias=cv("bpw2", oc), scale=1.0),
                      reads=[("B", bk)] + CONST_KEYS, writes=[("tmp", ti)])
                fw.op(DVE, lambda: nc.vector.scalar_tensor_tensor(out=Xc[:, oc, :T], in0=Xc[:, oc, :T], scalar=ALPHA,
                                                                  in1=tmps[ti][:, :T], op0=ALU.mult, op1=ALU.add),
                      reads=[("tmp", ti), xk(oc)], writes=[xk(oc)])
                ln_acc(Xc, xk, oc, T, first=(oc == 0))
            stream_group("pw2", T, NCH, g, grhs, gkeys, evac_pw2)
        if t["last"]:
            for (si_, _, _) in segs:
                if t["kind"] == "p":
                    dst = st_out_p[t["b"]]
                else:
                    dst = st_out_s[si_]
                fw.dma(SP, s_so, dst, ST[si_][:], reads=[("ST", si_)])
        if ti_ + 1 < len(tiles):
            tn = tiles[ti_ + 1]
            fw.dma(SP, s_x, C[:, :, :tn["T"]], x_src(tn), writes=[ck(c) for c in range(NCH)])
        ln_apply(Xc, xk, T, "mixg1", "mixb1",
                 [(AF.Identity, lambda c: Xc[:, c, :T], xk), (AF.Identity, lambda c: xb[:, c, :T], lambda c: ("xb", c))])

        rows = min(128, T)
        ntb = max(1, T // 128)
        o_wr, _ = _cv["wr"]
        bL = next_banks(1)[0]
        for tb in range(ntb):
            for c in range(NCH):
                fw.op(PE, lambda tb=tb, c=c: nc.tensor.matmul(banks[bL][0:rows, tb * NE:(tb + 1) * NE],
                                                              lhsT=Xc[:, c, tb * 128:tb * 128 + rows],
                                                              rhs=cvec[:, o_wr + c * NE:o_wr + (c + 1) * NE],
                                                              start=(c == 0), stop=(c == NCH - 1)),
                      reads=[xk(c)] + CONST_KEYS, writes=[("B", bL)], inc=(c == NCH - 1))
        fw.op(ACT, lambda: nc.scalar.copy(out=rt[0:rows, 0:ntb, :], in_=banks[bL][0:rows, 0:ntb * NE].rearrange("p (a e) -> p a e", e=NE)),
              reads=[("B", bL)], writes=[("rt",)])
        for tb in range(ntb):
            L = rt[0:rows, tb, :]
            L2 = rt2[0:rows, tb, :]
            Gt = rg[0:rows, tb, :]
            m1 = rs[0:rows, tb, 0:1]
            m2 = rs[0:rows, tb, 1:2]
            ssum = rs[0:rows, tb, 2:3]
            RK = [("rt",), ("rt2",), ("rg",), ("rs",)]
            dv = lambda fn: fw.op(DVE, fn, reads=RK, writes=RK[1:])
            dv(lambda: nc.vector.tensor_reduce(out=m1, in_=L, axis=mybir.AxisListType.X, op=ALU.max))
            dv(lambda: nc.vector.tensor_scalar(out=L2, in0=L, scalar1=m1, scalar2=None, op0=ALU.is_equal))
            dv(lambda: nc.vector.scalar_tensor_tensor(out=L2, in0=L2, scalar=-1e30, in1=L, op0=ALU.mult, op1=ALU.add))
            dv(lambda: nc.vector.tensor_reduce(out=m2, in_=L2, axis=mybir.AxisListType.X, op=ALU.max))
            dv(lambda: nc.vector.tensor_scalar(out=L2, in0=L, scalar1=m2, scalar2=None, op0=ALU.is_ge))
            dv(lambda: nc.vector.tensor_scalar(out=Gt, in0=L, scalar1=m1, scalar2=None, op0=ALU.subtract))
            fw.op(ACT, lambda: nc.scalar.activation(out=Gt, in_=Gt, func=AF.Exp), reads=RK, writes=RK[1:])
            dv(lambda: nc.vector.tensor_tensor(out=Gt, in0=Gt, in1=L2, op=ALU.mult))
            dv(lambda: nc.vector.tensor_reduce(out=ssum, in_=Gt, axis=mybir.AxisListType.X, op=ALU.add))
            dv(lambda: nc.vector.reciprocal(out=ssum, in_=ssum))
            dv(lambda: nc.vector.tensor_scalar(out=Gt, in0=Gt, scalar1=ssum, scalar2=None, op0=ALU.mult))
        bT = next_banks(1)[0]
        for tb in range(ntb):
            fw.op(PE, lambda tb=tb: nc.tensor.transpose(banks[bT][0:NE, tb * 128:tb * 128 + rows], rg[0:rows, tb, :],
                                                        ident[0:rows, 0:rows]),
                  reads=[("rg",), ("ident",)], writes=[("B", bT)])
        fw.op(ACT, lambda: nc.scalar.copy(out=gT[:, :T], in_=banks[bT][0:NE, :T]), reads=[("B", bT)], writes=[("gT",)])
        for e in range(NE):
            expert(T, xi, "moe_in", e * 11, "moe_out", e * 4, e, first=(e == 0), ln_final=(e == NE - 1))
        need_xb1 = False
        ln_apply(Xc, xk, T, "ffng1", "ffnb1",
                 [(AF.Identity, lambda c: Xc[:, c, :T], xk), (AF.Identity, lambda c: xb[:, c, :T], lambda c: ("xb", c))])
        ple(T, xi, 1, need_xb=need_xb1)
        def _store(t=t, Xc=Xc, T=T, xk=xk):
            fw.dma(SP, s_y, y_dst(t), Xc[:, :, :T], reads=[xk(c) for c in range(NCH)])
        if ti_ + 1 < len(tiles):
            after_wload.append(_store)
        else:
            _store()

    fw._emit_waits(SP, [(s_y.sem, s_y.count, "y"), (s_so.sem, s_so.count, "so")])
    es.close()
    return nc, fw


def _fm(v):
    v = np.asarray(v, np.float32)
    lead = v.shape[:-1]
    a = v.reshape(lead + (NCH, 128))
    return np.moveaxis(a, -1, 0)


def _build_cvec(inp):
    tab = np.zeros((128, NV), np.float32)

    def put(name, arr):
        o, w = _cv[name]
        tab[:, o:o + w] = np.asarray(arr, np.float32).reshape(128, w)

    put("convw", np.transpose(_fm(inp["rglru_conv_w"][0]), (0, 2, 1)))
    put("convb", _fm(inp["rglru_conv_b"][0]))
    put("ba", _fm(inp["rglru_b_a"][0]))
    put("bx", _fm(inp["rglru_b_x"][0]))
    put("lam", _fm(inp["rglru_lambda"][0]))
    put("bpw1", np.moveaxis(np.asarray(inp["cm_b_pw1"][0], np.float32).reshape(32, 128), -1, 0))
    put("dww", np.transpose(_fm(inp["cm_dw_w"][0]), (0, 2, 1)))
    put("dwb", _fm(inp["cm_dw_b"][0]))
    put("cmg", _fm(inp["cm_ln_g"][0]))
    put("cmb", _fm(inp["cm_ln_b"][0]))
    put("bpw2", _fm(inp["cm_b_pw2"][0]))
    for i in range(2):
        put(f"mixg{i}", _fm(inp["ln_mix_g"][i]))
        put(f"mixb{i}", _fm(inp["ln_mix_b"][i]))
        put(f"ffng{i}", _fm(inp["ln_ffn_g"][i]))
        put(f"ffnb{i}", _fm(inp["ln_ffn_b"][i]))
        put(f"pleb{i}", _fm(inp["ple_b_gate"][i]))
    wr = np.asarray(inp["moe_w_router"][0], np.float32).reshape(NCH, 128, NE)
    put("wr", np.transpose(wr, (1, 0, 2)))
    return tab


_PROG_CACHE = {}


def kernel(**inp):
    tile_sel = None
    if os.environ.get("MK_TILES"):
        tile_sel = [int(s) for s in os.environ["MK_TILES"].split(",")]
    key = tuple(tile_sel) if tile_sel else None
    if key not in _PROG_CACHE:
        _PROG_CACHE[key] = build_program(tile_sel)
    nc, fw = _PROG_CACHE[key]

    f32 = lambda a: np.asarray(a, np.float32)
    xp = f32(inp["x_prompt"])
    xs = f32(inp["x_sample"])
    pp = f32(inp["p_prompt"])
    ps = f32(inp["p_sample"])
    cvec = _build_cvec(inp)
    ident = np.eye(128, dtype=np.float32)
    selmat = np.zeros((NE, NE * 128), np.float32)
    for e in range(NE):
        selmat[e, e * 128:(e + 1) * 128] = 1.0
    shared = {
        "cvec": cvec, "ident": ident, "selmat": selmat,
        "rglru_w_in": f32(inp["rglru_w_in"][0]),
        "rglru_w_a": f32(inp["rglru_w_a"][0]).reshape(D, 256),
        "rglru_w_x": f32(inp["rglru_w_x"][0]).reshape(D, 256),
        "rglru_w_out": f32(inp["rglru_w_out"][0]),
        "cm_w_pw1": f32(inp["cm_w_pw1"][0]),
        "cm_w_pw2": f32(inp["cm_w_pw2"][0]),
        "ffn_w_in": f32(inp["ffn_w_in"][0]),
        "ffn_w_out": f32(inp["ffn_w_out"][0]),
        "moe_w_in": f32(inp["moe_w_in"][0]).reshape(NE * D, 2 * EFF),
        "moe_w_out": f32(inp["moe_w_out"][0]).reshape(NE * EFF, D),
        "ple_w_proj0": f32(inp["ple_w_proj"][0]), "ple_w_proj1": f32(inp["ple_w_proj"][1]),
        "ple_w_gate0": f32(inp["ple_w_gate"][0]), "ple_w_gate1": f32(inp["ple_w_gate"][1]),
    }
    st_tok = np.concatenate([f32(inp["state_rglru_conv"][0]), f32(inp["state_rglru_h"][0])[:, None, :],
                             f32(inp["state_conv_module"][0])], axis=1)
    st_fm = np.ascontiguousarray(st_tok.reshape(16, NST, NCH, 128).transpose(0, 3, 2, 1))
    in_maps = []
    for c in range(NCORES):
        b0 = 2 * c
        xTp = np.ascontiguousarray(xp[b0:b0 + 2].reshape(2, SEQ, NCH, 128).transpose(0, 3, 2, 1))
        xTs = np.ascontiguousarray(xs[b0:b0 + 2].reshape(2 * DEC, NCH, 128).transpose(2, 1, 0))
        pTp = np.ascontiguousarray(pp[:, b0:b0 + 2].reshape(2, 2, SEQ, 2, 128).transpose(0, 1, 4, 3, 2))
        pTs = np.ascontiguousarray(ps[:, b0:b0 + 2].reshape(2, 2 * DEC, 2, 128).transpose(0, 3, 2, 1))
        m = dict(shared)
        m.update({"xTp": xTp, "xTs": xTs, "pTp": pTp, "pTs": pTs, "st_in": st_fm[b0:b0 + 2]})
        in_maps.append(m)
    res = run_bass_kernel_spmd(nc, in_maps, core_ids=list(range(NCORES)))
    R = res.results
    yTp = np.stack([r["yTp"] for r in R])
    y_prompt = np.ascontiguousarray(yTp.transpose(0, 1, 4, 3, 2)).reshape(16, SEQ, D)
    yTs = np.stack([r["yTs"] for r in R])
    y_sample = np.ascontiguousarray(yTs.transpose(0, 3, 2, 1)).reshape(16, DEC, D)

    def split_state(key):
        s = np.stack([r[key] for r in R]).reshape(16, 128, NCH, NST)
        tok = np.ascontiguousarray(s.transpose(0, 3, 2, 1)).reshape(16, NST, D)
        return (np.ascontiguousarray(tok[None, :, 0:3, :]), np.ascontiguousarray(tok[None, :, 3, :]),
                np.ascontiguousarray(tok[None, :, 4:34, :]))
    cp, hp, cmp_ = split_state("st_out_p")
    cs, hs, cms = split_state("st_out_s")
    return (y_prompt, y_sample, cp, hp, cmp_, cs, hs, cms)
```

```python
import os
from contextlib import ExitStack

import numpy as np
import concourse.bass as bass
import concourse.mybir as mybir
from concourse.bass_utils import run_bass_kernel_spmd

F32 = mybir.dt.float32
BF16 = mybir.dt.bfloat16
AF = mybir.ActivationFunctionType
ALU = mybir.AluOpType

NCORES = 8
D = 2048
NCH = 16
SEQ = 2048
TT = 512
DEC = 16
PLE = 256
DFF = 5632
EFF = 2816
NE = 8
ALPHA = (2.0 * 2) ** 0.25
EPS = 1e-5
NST = 34
KB = 16
NSLOT = 2
NTMP = 8

_cv = {}
_off = 0
for _n, _w in [("convw", 64), ("convb", 16), ("ba", 16), ("bx", 16), ("lam", 16),
               ("bpw1", 32), ("dww", 496), ("dwb", 16), ("cmg", 16), ("cmb", 16), ("bpw2", 16),
               ("mixg0", 16), ("mixb0", 16), ("mixg1", 16), ("mixb1", 16),
               ("ffng0", 16), ("ffnb0", 16), ("ffng1", 16), ("ffnb1", 16),
               ("pleb0", 16), ("pleb1", 16), ("wr", 128), ("cl", 16), ("cl2", 16)]:
    _cv[_n] = (_off, _w)
    _off += _w
NV = _off


class _Eng:
    def __init__(self, name, eng, sem, self_sync):
        self.name, self.eng, self.sem, self.self_sync = name, eng, sem, self_sync
        self.count = 0
        self.waited = {}


class _Stream:
    def __init__(self, name, sem):
        self.name, self.sem, self.count = name, sem, 0


class _FW:
    def __init__(self):
        self.lastw = {}
        self.readers = {}
        self.nwait = 0
        self.ninst = 0

    def _deps(self, reads, writes):
        deps = []
        for k in reads:
            t = self.lastw.get(k)
            if t is not None:
                deps.append(t)
        for k in writes:
            t = self.lastw.get(k)
            if t is not None:
                deps.append(t)
            deps.extend(self.readers.get(k, ()))
        return deps

    def _emit_waits(self, E, deps):
        best = {}
        for (sem, val, who) in deps:
            if who == E.name and not E.self_sync:
                continue
            if E.waited.get(id(sem), 0) >= val:
                continue
            if best.get(id(sem), (None, 0))[1] < val:
                best[id(sem)] = (sem, val)
        for sem, val in best.values():
            E.eng.wait_ge(sem, val)
            E.waited[id(sem)] = val
            self.nwait += 1

    def _record(self, tok, reads, writes):
        for k in writes:
            self.lastw[k] = tok
            self.readers[k] = []
        for k in reads:
            self.readers.setdefault(k, []).append(tok)

    def op(self, E, fn, reads=(), writes=(), inc=True):
        self._emit_waits(E, self._deps(reads, writes))
        inst = fn()
        self.ninst += 1
        if inc:
            inst.then_inc(E.sem, 1)
            E.count += 1
            tok = (E.sem, E.count, E.name)
        else:
            tok = (E.sem, E.count + 1, E.name)
        self._record(tok, reads, writes)

    def dma(self, Q, S, out, in_, reads=(), writes=(), **kw):
        self._emit_waits(Q, self._deps(reads, writes))
        inst = Q.eng.dma_start(out=out, in_=in_, **kw)
        self.ninst += 1
        inst.then_inc(S.sem, 16)
        S.count += 16
        tok = (S.sem, S.count, S.name)
        self._record(tok, reads, writes)


def build_program(tile_sel=None):
    nc = bass.Bass("TRN2", target_bir_lowering=False)
    es = ExitStack()

    def din(name, shape, dt=F32):
        return nc.dram_tensor(name, list(shape), dt, kind="ExternalInput").ap()

    def dout(name, shape):
        return nc.dram_tensor(name, list(shape), F32, kind="ExternalOutput").ap()

    def dscr(name, shape):
        return nc.dram_tensor(name, list(shape), BF16, kind="Internal").ap()

    xTp = din("xTp", [2, 128, NCH, SEQ])
    xTs = din("xTs", [128, NCH, 2 * DEC])
    pTp = din("pTp", [2, 2, 128, 2, SEQ])
    pTs = din("pTs", [2, 128, 2, 2 * DEC])
    st_in = din("st_in", [2, 128, NCH, NST])
    cvec_d = din("cvec", [128, NV])
    ident_d = din("ident", [128, 128])
    selmat_d = din("selmat", [NE, NE * 128])
    wsrc = {
        "rg_in": din("rglru_w_in", [D, 2 * D]),
        "rg_a": din("rglru_w_a", [D, 256]),
        "rg_x": din("rglru_w_x", [D, 256]),
        "rg_out": din("rglru_w_out", [D, D]),
        "pw1": din("cm_w_pw1", [D, 2 * D]),
        "pw2": din("cm_w_pw2", [D, D]),
        "ffn_in": din("ffn_w_in", [D, 2 * DFF]),
        "ffn_out": din("ffn_w_out", [DFF, D]),
        "moe_in": din("moe_w_in", [NE * D, 2 * EFF]),
        "moe_out": din("moe_w_out", [NE * EFF, D]),
        "pproj0": din("ple_w_proj0", [PLE, D]),
        "pproj1": din("ple_w_proj1", [PLE, D]),
        "pgate0": din("ple_w_gate0", [D, D]),
        "pgate1": din("ple_w_gate1", [D, D]),
    }
    WSPEC = {
        "rg_in": ("plain", 1, D, 8), "rg_out": ("plain", 1, D, 4), "pw2": ("plain", 1, D, 4),
        "pgate0": ("plain", 1, D, 4), "pgate1": ("plain", 1, D, 4),
        "pproj0": ("plain", 1, PLE, 4), "pproj1": ("plain", 1, PLE, 4),
        "ffn_out": ("plain", 2, EFF, 4), "moe_out": ("plain", NE, EFF, 4),
        "pw1": ("paired", 1, D, 8), "ffn_in": ("paired", 1, D, 22), "moe_in": ("paired", NE, D, 11),
    }
    wbf = {k: dscr("bf_" + k, [S * Gs, 128, rows // 128, 512]) for k, (kind, S, rows, Gs) in WSPEC.items()}
    wbf["rg_ax"] = dscr("bf_rg_ax", [1, 128, NCH, 512])
    yTp = dout("yTp", [2, 128, NCH, SEQ])
    yTs = dout("yTs", [128, NCH, 2 * DEC])
    st_out_p = dout("st_out_p", [2, 128, NCH, NST])
    st_out_s = dout("st_out_s", [2, 128, NCH, NST])

    def sb(name, shape, dt=F32):
        return es.enter_context(nc.sbuf_tensor(name, list(shape), dt))

    X = [sb("X0", [128, NCH, TT]), sb("X1", [128, NCH, TT])]
    xb = sb("xb", [128, NCH, TT], BF16)
    G = sb("G", [128, NCH, TT], BF16)
    H = sb("H", [128, 22, TT], BF16)
    Wsl = [sb(f"W{i}", [128, KB, 512], BF16) for i in range(NSLOT)]
    tmps = [sb(f"tmp{i}", [128, TT]) for i in range(NTMP)]
    cbufs = [sb(f"cb{i}", [128, 544]) for i in range(2)]
    pland = [sb("pl0", [128, 2, TT])] * 2
    gbuf = [sb(f"gbuf{i}", [128, TT]) for i in range(2)]
    lnm = sb("lnm", [128, TT])
    lnv = sb("lnv", [128, TT])
    lns1 = sb("lns1", [128, TT])
    lns2 = sb("lns2", [128, TT])
    pbf = [sb(f"pb{i}", [128, 2, TT], BF16) for i in range(2)]
    ST = [sb(f"ST{i}", [128, NCH, NST]) for i in range(2)]
    cvec = sb("cvec_sb", [128, NV])
    ident = sb("ident_sb", [128, 128])
    onesD = sb("onesD", [128, 128])
    selmat = sb("selmat_sb", [NE, NE * 128])
    rt = sb("rt", [128, 4, NE])
    rg = sb("rgates", [128, 4, NE])
    rs = sb("rsmall", [128, 4, 8])
    rt2 = sb("rt2", [128, 4, NE])
    gT = sb("gT", [NE, TT])
    banks = [es.enter_context(nc.psum_tensor(f"ps{i}", [128, 512], F32)) for i in range(8)]

    def sem(name):
        return es.enter_context(nc.semaphore(name))

    PE = _Eng("pe", nc.tensor, sem("s_pe"), False)
    ACT = _Eng("act", nc.scalar, sem("s_act"), True)
    DVE = _Eng("dve", nc.vector, sem("s_dve"), True)
    POOL = _Eng("pool", nc.gpsimd, sem("s_pool"), True)
    SP = _Eng("sp", nc.sync, sem("s_sp"), True)
    s_w = [_Stream(f"w{i}", sem(f"s_w{i}")) for i in range(NSLOT)]
    s_x = _Stream("x", sem("s_x"))
    s_p = [_Stream(f"p{i}", sem(f"s_p{i}")) for i in range(2)]
    s_st = _Stream("st", sem("s_st"))
    s_y = _Stream("y", sem("s_y"))
    s_so = _Stream("so", sem("s_so"))
    s_c = _Stream("c", sem("s_c"))
    s_castA = _Stream("castA", sem("s_castA"))
    s_castB = _Stream("castB", sem("s_castB"))
    s_castC = _Stream("castC", sem("s_castC"))
    s_castD = _Stream("castD", sem("s_castD"))

    fw = _FW()
    state = {"bank": 0, "tmp": 0, "slot": 0, "cb": 0, "use_pool": False}

    def cv(name, c=None, k=None):
        o, w = _cv[name]
        if k is not None:
            return cvec[:, o + k:o + k + 1]
        if c is not None:
            return cvec[:, o + c:o + c + 1]
        return cvec[:, o:o + w]

    def next_banks(n):
        b0 = state["bank"]
        if b0 + n > 8:
            b0 = 0
        state["bank"] = (b0 + n) % 8
        return list(range(b0, b0 + n))

    def next_tmp():
        i = state["tmp"]
        state["tmp"] = (i + 1) % NTMP
        return i

    def next_cb():
        i = state["cb"]
        state["cb"] = (i + 1) % 2
        return i

    fw.dma(SP, s_c, cvec[:], cvec_d, writes=[("cvec",)])
    fw.dma(SP, s_c, ident[:], ident_d, writes=[("ident",)])
    fw.dma(SP, s_c, selmat[:], selmat_d, writes=[("selmat",)])
    for _k in (("cvec",), ("ident",), ("selmat",)):
        fw.lastw[_k] = (s_c.sem, s_c.count, "c")
    fw.op(DVE, lambda: nc.vector.memset(onesD[:], 1.0 / D), writes=[("onesD",)])
    o_lam, _ = _cv["lam"]
    o_cl, _ = _cv["cl"]
    o_cl2, _ = _cv["cl2"]
    fw.op(ACT, lambda: nc.scalar.activation(out=cvec[:, o_cl:o_cl + 16], in_=cvec[:, o_lam:o_lam + 16],
                                            func=AF.Exp, scale=-1.0), reads=[("cvec",)], writes=[("cl",)])
    fw.op(ACT, lambda: nc.scalar.activation(out=cvec[:, o_cl:o_cl + 16], in_=cvec[:, o_cl:o_cl + 16],
                                            func=AF.Ln, bias=1.0, scale=1.0), reads=[("cl",)], writes=[("cl",)])
    fw.op(ACT, lambda: nc.scalar.mul(out=cvec[:, o_cl2:o_cl2 + 16], in_=cvec[:, o_cl:o_cl + 16], mul=-16.0),
          reads=[("cl",)], writes=[("cl2",)])
    fw.op(ACT, lambda: nc.scalar.mul(out=cvec[:, o_cl:o_cl + 16], in_=cvec[:, o_cl:o_cl + 16], mul=-8.0),
          reads=[("cl",), ("cl2",)], writes=[("cl",)])
    CONST_KEYS = [("cvec",), ("cl",), ("cl2",)]

    tiles = []
    for b in range(2):
        for j in range(SEQ // TT):
            tiles.append(dict(kind="p", T=TT, segs=[(0, 0, TT)], b=b, t0=j * TT,
                              first=(j == 0), last=(j == SEQ // TT - 1)))
    tiles.append(dict(kind="s", T=2 * DEC, segs=[(0, 0, DEC), (1, DEC, DEC)], first=True, last=True))
    if tile_sel is not None:
        tiles = [tiles[i] for i in tile_sel]

    def x_src(t):
        if t["kind"] == "p":
            return xTp[t["b"], :, :, t["t0"]:t["t0"] + TT]
        return xTs

    def y_dst(t):
        if t["kind"] == "p":
            return yTp[t["b"], :, :, t["t0"]:t["t0"] + TT]
        return yTs

    def p_src(t, layer):
        if t["kind"] == "p":
            return pTp[layer, t["b"], :, :, t["t0"]:t["t0"] + TT]
        return pTs[layer]

    def xkeys(buf_i):
        return [("X", buf_i, c) for c in range(NCH)]

    t0 = tiles[0]
    fw.dma(SP, s_x, X[0][:, :, :t0["T"]], x_src(t0), writes=xkeys(0))

    cast_streams = [s_castA, s_castB, s_castC, s_castD]
    cast_toks = {}
    ncast = [0]

    def cast_dma(dv, sv):
        S = cast_streams[ncast[0] % len(cast_streams)]
        ncast[0] += 1
        if S.count:
            fw._emit_waits(POOL, [(S.sem, S.count, S.name)])
        inst = nc.gpsimd.dma_start(out=dv, in_=sv)
        inst.then_inc(S.sem, 16)
        S.count += 16
        fw.ninst += 1

    def cast_group(name, idx):
        kind, S, rows, Gs = WSPEC[name]
        src = wsrc[name]
        N = src.shape[1]
        sp, g = divmod(idx, Gs)
        r0 = sp * rows
        if kind == "plain":
            sv = src[r0:r0 + rows, g * 512:(g + 1) * 512].rearrange("(k p) n -> p k n", p=128)
            cast_dma(wbf[name][idx], sv)
        else:
            for hsel in range(2):
                c0 = hsel * (N // 2) + g * 256
                sv = src[r0:r0 + rows, c0:c0 + 256].rearrange("(k p) n -> p k n", p=128)
                cast_dma(wbf[name][idx][:, :, hsel * 256:(hsel + 1) * 256], sv)
        cast_toks[(name, idx)] = [(St.sem, St.count, St.name) for St in cast_streams if St.count]

    def cast_all(name, idxs=None):
        kind, S, rows, Gs = WSPEC[name]
        for idx in (range(S * Gs) if idxs is None else idxs):
            cast_group(name, idx)

    cast_all("rg_in", [4, 5, 6, 7, 0, 1, 2, 3])
    for hsel, nm in enumerate(("rg_a", "rg_x")):
        cast_dma(wbf["rg_ax"][0][:, :, hsel * 256:(hsel + 1) * 256],
                 wsrc[nm].rearrange("(k p) n -> p k n", p=128))
    cast_toks[("rg_ax", 0)] = [(St.sem, St.count, St.name) for St in cast_streams if St.count]
    cast_all("rg_out")
    for hf in range(2):
        cast_all("ffn_in", range(hf * 11, hf * 11 + 11))
        cast_all("ffn_out", range(hf * 4, hf * 4 + 4))
    for g in range(4):
        cast_group("pgate0", g)
        cast_group("pproj0", g)
    cast_all("pw1")
    cast_all("pw2")
    for e in range(NE):
        cast_all("moe_in", range(e * 11, e * 11 + 11))
        cast_all("moe_out", range(e * 4, e * 4 + 4))
    for g in range(4):
        cast_group("pgate1", g)
        cast_group("pproj1", g)

    after_wload = []

    def wload(name, idx, kc0, nk):
        si = state["slot"]
        state["slot"] = (si + 1) % NSLOT
        key = ("W", si)
        deps = fw._deps((), [key]) + cast_toks[(name, idx)]
        fw._emit_waits(SP, deps)
        inst = nc.sync.dma_start(out=Wsl[si][:, 0:nk, :], in_=wbf[name][idx, :, kc0:kc0 + nk, :])
        inst.then_inc(s_w[si].sem, 16)
        s_w[si].count += 16
        fw.ninst += 1
        fw._record((s_w[si].sem, s_w[si].count, s_w[si].name), (), [key])
        while after_wload:
            after_wload.pop(0)()
        return si

    def stream_group(name, T, nkc, idx, rhs_fn, rhs_keys_fn, evac_fn):
        noc = 4
        bks = next_banks(noc)
        nblk = -(-nkc // KB)
        bs = -(-nkc // nblk)
        for kb0 in range(0, nkc, bs):
            nk = min(bs, nkc - kb0)
            si = wload(name, idx, kb0, nk)
            for oi in range(noc):
                for kk in range(nk):
                    kc = kb0 + kk
                    last = (kk == nk - 1)
                    fw.op(PE, (lambda oi=oi, kk=kk, kc=kc, si=si:
                               nc.tensor.matmul(banks[bks[oi]][:, :T], lhsT=Wsl[si][:, kk, oi * 128:(oi + 1) * 128],
                                                rhs=rhs_fn(kc), start=(kc == 0), stop=(kc == nkc - 1))),
                          reads=[("W", si)] + rhs_keys_fn(kc), writes=[("B", bks[oi])], inc=last)
        evac_fn(bks)

    def xb_rhs(T):
        return (lambda kc: xb[:, kc, :T]), (lambda kc: [("xb", kc)])

    S1K, S2K = ("lns1",), ("lns2",)

    def ln_acc(src, srckey, c, T, first):
        if first:
            fw.op(DVE, lambda: nc.vector.tensor_copy(out=lns1[:, :T], in_=src[:, c, :T]),
                  reads=[srckey(c)], writes=[S1K])
            fw.op(ACT, lambda: nc.scalar.activation(out=lns2[:, :T], in_=src[:, c, :T], func=AF.Square),
                  reads=[srckey(c)], writes=[S2K])
        else:
            fw.op(DVE, lambda: nc.vector.tensor_tensor(out=lns1[:, :T], in0=lns1[:, :T], in1=src[:, c, :T], op=ALU.add),
                  reads=[srckey(c), S1K], writes=[S1K])
            ti = next_tmp()
            fw.op(ACT, lambda: nc.scalar.activation(out=tmps[ti][:, :T], in_=src[:, c, :T], func=AF.Square),
                  reads=[srckey(c)], writes=[("tmp", ti)])
            fw.op(DVE, lambda: nc.vector.tensor_tensor(out=lns2[:, :T], in0=lns2[:, :T], in1=tmps[ti][:, :T], op=ALU.add),
                  reads=[("tmp", ti), S2K], writes=[S2K])

    def ln_stats(src, srckey, T):
        bA, bB = next_banks(2)
        fw.op(PE, lambda: nc.tensor.matmul(banks[bA][:, :T], lhsT=onesD[:], rhs=lns1[:, :T], start=True, stop=True),
              reads=[("onesD",), S1K], writes=[("B", bA)])
        fw.op(PE, lambda: nc.tensor.matmul(banks[bB][:, :T], lhsT=onesD[:], rhs=lns2[:, :T], start=True, stop=True),
              reads=[("onesD",), S2K], writes=[("B", bB)])
        LM, LV = ("lnm",), ("lnv",)
        fw.op(ACT, lambda: nc.scalar.copy(out=lnm[:, :T], in_=banks[bA][:, :T]),
              reads=[("B", bA)], writes=[LM])
        fw.op(DVE, lambda: nc.vector.tensor_tensor(out=lnv[:, :T], in0=lnm[:, :T], in1=lnm[:, :T],
                                                   op=ALU.mult), reads=[LM], writes=[LV])
        fw.op(DVE, lambda: nc.vector.tensor_tensor(out=lnv[:, :T], in0=banks[bB][:, :T], in1=lnv[:, :T],
                                                   op=ALU.subtract), reads=[("B", bB), LV], writes=[LV])
        fw.op(DVE, lambda: nc.vector.tensor_scalar_max(out=lnv[:, :T], in0=lnv[:, :T], scalar1=0.0),
              reads=[LV], writes=[LV])
        fw.op(ACT, lambda: nc.scalar.activation(out=lnv[:, :T], in_=lnv[:, :T], func=AF.Sqrt,
                                                bias=EPS, scale=1.0), reads=[LV], writes=[LV])
        fw.op(DVE, lambda: nc.vector.reciprocal(out=lnv[:, :T], in_=lnv[:, :T]),
              reads=[LV], writes=[LV])
        fw.op(DVE, lambda: nc.vector.scalar_tensor_tensor(out=lnm[:, :T], in0=lnm[:, :T], scalar=-1.0,
                                                          in1=lnv[:, :T], op0=ALU.mult, op1=ALU.mult),
              reads=[LM, LV], writes=[LM])
        return LM, LV

    def ln_apply(src, srckey, T, gname, bname, out_specs):
        LM, LV = ln_stats(src, srckey, T)
        for c0 in range(0, NCH, 2):
            tis = [next_tmp(), next_tmp()]
            for q in range(2):
                c, ti = c0 + q, tis[q]
                fw.op(DVE, lambda c=c, ti=ti: nc.vector.tensor_tensor(out=tmps[ti][:, :T], in0=src[:, c, :T],
                                                                      in1=lnv[:, :T], op=ALU.mult),
                      reads=[srckey(c), LV], writes=[("tmp", ti)])
            for q in range(2):
                ti = tis[q]
                fw.op(DVE, lambda ti=ti: nc.vector.tensor_tensor(out=tmps[ti][:, :T], in0=tmps[ti][:, :T],
                                                                 in1=lnm[:, :T], op=ALU.add),
                      reads=[("tmp", ti), LM], writes=[("tmp", ti)])
            for q in range(2):
                c, ti = c0 + q, tis[q]
                for oi_, (func, dst_fn, key_fn) in enumerate(out_specs):
                    if oi_ == 1 and state["use_pool"] and func == AF.Identity and c % 2 == 0:
                        fw.op(POOL, lambda c=c, ti=ti, dst_fn=dst_fn:
                              nc.gpsimd.tensor_scalar(out=dst_fn(c), in0=tmps[ti][:, :T], scalar1=cv(gname, c),
                                                      scalar2=cv(bname, c), op0=ALU.mult, op1=ALU.add),
                              reads=[("tmp", ti)] + CONST_KEYS, writes=[key_fn(c)])
                        continue
                    fw.op(ACT, lambda c=c, ti=ti, func=func, dst_fn=dst_fn:
                          nc.scalar.activation(out=dst_fn(c), in_=tmps[ti][:, :T], func=func,
                                               bias=cv(bname, c), scale=cv(gname, c)),
                          reads=[("tmp", ti)] + CONST_KEYS, writes=[key_fn(c)])

    def expert(T, xi, in_name, in_base, out_name, out_base, gate_e, first, ln_final=False):
        xk = lambda c: ("X", xi, c)
        rhs, rkeys = xb_rhs(T)
        for j in range(EFF // 256):
            def evac(bks, j=j):
                for oi in range(2):
                    bk, bu = bks[oi], bks[oi + 2]
                    ti = next_tmp()
                    fw.op(ACT, lambda: nc.scalar.activation(out=tmps[ti][:, :T], in_=banks[bk][:, :T], func=AF.Silu),
                          reads=[("B", bk)], writes=[("tmp", ti)])
                    fw.op(DVE, lambda: nc.vector.tensor_tensor(out=H[:, 2 * j + oi, :T], in0=banks[bu][:, :T],
                                                               in1=tmps[ti][:, :T], op=ALU.mult),
                          reads=[("B", bu), ("tmp", ti)], writes=[("H", 2 * j + oi)])
            stream_group(in_name, T, NCH, in_base + j, rhs, rkeys, evac)
        gi = None
        if gate_e is not None:
            gi = gate_e % 2
            bg = next_banks(1)[0]
            fw.op(PE, lambda: nc.tensor.matmul(banks[bg][:, :T], lhsT=selmat[:, gate_e * 128:(gate_e + 1) * 128],
                                               rhs=gT[:, :T], start=True, stop=True),
                  reads=[("selmat",), ("gT",)], writes=[("B", bg)])
            fw.op(ACT, lambda: nc.scalar.copy(out=gbuf[gi][:, :T], in_=banks[bg][:, :T]),
                  reads=[("B", bg)], writes=[("gbuf", gi)])
        hrhs = lambda kc: H[:, kc, :T]
        hkeys = lambda kc: [("H", kc)]
        for g in range(4):
            def evac(bks, g=g):
                for oi, bk in enumerate(bks):
                    oc = g * 4 + oi
                    src_ap = banks[bk][:, :T]
                    rk = [("B", bk)]
                    if gate_e is not None:
                        ti = next_tmp()
                        fw.op(DVE, lambda: nc.vector.tensor_tensor(out=tmps[ti][:, :T], in0=banks[bk][:, :T],
                                                                   in1=gbuf[gi][:, :T], op=ALU.mult),
                              reads=[("B", bk), ("gbuf", gi)], writes=[("tmp", ti)])
                        src_ap = tmps[ti][:, :T]
                        rk = [("tmp", ti)]
                    if first:
                        fw.op(DVE, lambda: nc.vector.scalar_tensor_tensor(out=X[xi][:, oc, :T], in0=X[xi][:, oc, :T],
                                                                          scalar=ALPHA, in1=src_ap, op0=ALU.mult, op1=ALU.add),
                              reads=rk + [xk(oc)], writes=[xk(oc)])
                    else:
                        fw.op(DVE, lambda: nc.vector.tensor_tensor(out=X[xi][:, oc, :T], in0=X[xi][:, oc, :T],
                                                                   in1=src_ap, op=ALU.add),
                              reads=rk + [xk(oc)], writes=[xk(oc)])
                    if ln_final:
                        ln_acc(X[xi], xk, oc, T, first=(oc == 0))
            stream_group(out_name, T, EFF // 128, out_base + g, hrhs, hkeys, evac)

    def ple(T, xi, layer, need_xb):
        xk = lambda c: ("X", xi, c)
        rhs, rkeys = xb_rhs(T)
        prhs = lambda kc: pbf[layer][:, kc, :T]
        pkeys = lambda kc: [("pbf", layer)]
        for g in range(4):
            sig = {}

            def evac_gate(bks, g=g, sig=sig):
                for oi, bk in enumerate(bks):
                    oc = g * 4 + oi
                    ti = next_tmp()
                    fw.op(ACT, lambda: nc.scalar.activation(out=tmps[ti][:, :T], in_=banks[bk][:, :T], func=AF.Sigmoid,
                                                            bias=cv(f"pleb{layer}", oc), scale=1.0),
                          reads=[("B", bk)] + CONST_KEYS, writes=[("tmp", ti)])
                    sig[oi] = ti

            def evac_proj(bks, g=g, sig=sig):
                for oi, bk in enumerate(bks):
                    oc = g * 4 + oi
                    ti = sig[oi]
                    fw.op(DVE, lambda: nc.vector.tensor_tensor(out=tmps[ti][:, :T], in0=banks[bk][:, :T],
                                                               in1=tmps[ti][:, :T], op=ALU.mult),
                          reads=[("B", bk), ("tmp", ti)], writes=[("tmp", ti)])
                    fw.op(DVE, lambda: nc.vector.tensor_tensor(out=X[xi][:, oc, :T], in0=X[xi][:, oc, :T],
                                                               in1=tmps[ti][:, :T], op=ALU.add),
                          reads=[("tmp", ti), xk(oc)], writes=[xk(oc)])
            stream_group(f"pgate{layer}", T, NCH, g, rhs, rkeys, evac_gate)
            stream_group(f"pproj{layer}", T, 2, g, prhs, pkeys, evac_proj)
        if need_xb:
            for oc in range(NCH):
                if oc % 2:
                    fw.op(ACT, lambda: nc.scalar.copy(out=xb[:, oc, :T], in_=X[xi][:, oc, :T]),
                          reads=[xk(oc)], writes=[("xb", oc)])
                else:
                    fw.op(DVE, lambda: nc.vector.tensor_copy(out=xb[:, oc, :T], in_=X[xi][:, oc, :T]),
                          reads=[xk(oc)], writes=[("xb", oc)])

    for ti_, t in enumerate(tiles):
        T = t["T"]
        xi = ti_ % 2
        ci = 1 - xi
        Xc = X[xi]
        C = X[ci]
        xk = lambda c, xi=xi: ("X", xi, c)
        ck = lambda c, ci=ci: ("X", ci, c)
        segs = t["segs"]
        state["use_pool"] = (ti_ >= 1)

        if t["first"]:
            if t["kind"] == "p":
                fw.op(DVE, lambda: nc.vector.memset(ST[0][:], 0.0), writes=[("ST", 0)])
            else:
                for (si_, _, _) in segs:
                    fw.dma(SP, s_st, ST[si_][:], st_in[si_], writes=[("ST", si_)])
                for (si_, _, _) in segs:
                    fw.lastw[("ST", si_)] = (s_st.sem, s_st.count, "st")
        for layer in range(2):
            fw.dma(SP, s_p[layer], pland[layer][:, :, :T], p_src(t, layer), writes=[("pland",)])
            fw.op(DVE, lambda layer=layer: nc.vector.tensor_copy(out=pbf[layer][:, :, :T], in_=pland[layer][:, :, :T]),
                  reads=[("pland",)], writes=[("pbf", layer)])
        for c in range(NCH):
            E = ACT if c % 2 else DVE
            if E is ACT:
                fw.op(ACT, lambda c=c: nc.scalar.copy(out=xb[:, c, :T], in_=Xc[:, c, :T]),
                      reads=[xk(c)], writes=[("xb", c)])
            else:
                fw.op(DVE, lambda c=c: nc.vector.tensor_copy(out=xb[:, c, :T], in_=Xc[:, c, :T]),
                      reads=[xk(c)], writes=[("xb", c)])

        rhs, rkeys = xb_rhs(T)
        for g in range(4):
            def evac_rec(bks, g=g):
              for oi, bk in enumerate(bks):
                c = g * 4 + oi
                for (si_, col0, ln) in segs:
                    cbi = next_cb()
                    cb = cbufs[cbi]
                    fw.op(DVE, lambda: nc.vector.tensor_copy(out=cb[:, 0:3], in_=ST[si_][:, c, 0:3]),
                          reads=[("ST", si_)], writes=[("cb", cbi)])
                    fw.op(ACT, lambda: nc.scalar.copy(out=cb[:, 3:3 + ln], in_=banks[bk][:, col0:col0 + ln]),
                          reads=[("B", bk), ("cb", cbi)], writes=[("cb", cbi)])
                    xc = C[:, c, col0:col0 + ln]
                    ta = next_tmp()
                    a1 = tmps[ta][:, :ln]
                    fw.op(DVE, lambda: nc.vector.tensor_scalar(out=xc, in0=cb[:, 0:ln], scalar1=cv("convw", k=c * 4 + 0),
                                                               scalar2=cv("convb", c), op0=ALU.mult, op1=ALU.add),
                          reads=[("cb", cbi)] + CONST_KEYS, writes=[ck(c)])
                    fw.op(DVE, lambda: nc.vector.tensor_scalar(out=a1, in0=cb[:, 1:1 + ln], scalar1=cv("convw", k=c * 4 + 1),
                                                               scalar2=None, op0=ALU.mult),
                          reads=[("cb", cbi)] + CONST_KEYS, writes=[("tmp", ta)])
                    fw.op(DVE, lambda: nc.vector.scalar_tensor_tensor(out=xc, in0=cb[:, 2:2 + ln],
                                                                      scalar=cv("convw", k=c * 4 + 2), in1=xc,
                                                                      op0=ALU.mult, op1=ALU.add),
                          reads=[("cb", cbi), ck(c)] + CONST_KEYS, writes=[ck(c)])
                    fw.op(DVE, lambda: nc.vector.scalar_tensor_tensor(out=a1, in0=cb[:, 3:3 + ln],
                                                                      scalar=cv("convw", k=c * 4 + 3), in1=a1,
                                                                      op0=ALU.mult, op1=ALU.add),
                          reads=[("cb", cbi), ("tmp", ta)] + CONST_KEYS, writes=[("tmp", ta)])
                    fw.op(DVE, lambda: nc.vector.tensor_tensor(out=xc, in0=xc, in1=a1, op=ALU.add),
                          reads=[ck(c), ("tmp", ta)], writes=[ck(c)])
                    fw.op(ACT, lambda: nc.scalar.copy(out=ST[si_][:, c, 0:3], in_=cb[:, ln:ln + 3]),
                          reads=[("cb", cbi), ("ST", si_)], writes=[("ST", si_)])
                if state["use_pool"]:
                    fw.op(POOL, lambda: nc.gpsimd.tensor_copy(out=H[:, c, :T], in_=C[:, c, :T]),
                          reads=[ck(c)], writes=[("H", c)])
                else:
                    fw.op(ACT, lambda: nc.scalar.copy(out=H[:, c, :T], in_=C[:, c, :T]),
                          reads=[ck(c)], writes=[("H", c)])
            stream_group("rg_in", T, NCH, 4 + g, rhs, rkeys, evac_rec)
        for g in range(4):
            def evac_gate(bks, g=g):
                for oi, bk in enumerate(bks):
                    c = g * 4 + oi
                    fw.op(ACT, lambda: nc.scalar.activation(out=G[:, c, :T], in_=banks[bk][:, :T], func=AF.Gelu),
                          reads=[("B", bk)], writes=[("G", c)])
            stream_group("rg_in", T, NCH, g, rhs, rkeys, evac_gate)

        for kb0 in range(0, NCH, KB):
            si_w = wload("rg_ax", 0, kb0, KB)
            for hh in range(KB // 2):
                h = kb0 // 2 + hh
                for j in range(2):
                    oc = 2 * h + j
                    bA, bX = next_banks(2)
                    for (bk, coff) in ((bA, 0), (bX, 256)):
                        for i in range(2):
                            kk = 2 * hh + i
                            fw.op(PE, lambda bk=bk, coff=coff, kk=kk, i=i, j=j, h=h:
                                  nc.tensor.matmul(banks[bk][:, :T], lhsT=Wsl[si_w][:, kk, coff + j * 128:coff + (j + 1) * 128],
                                                   rhs=H[:, 2 * h + i, :T], start=(i == 0), stop=(i == 1)),
                                  reads=[("W", si_w), ("H", 2 * h + i)], writes=[("B", bk)], inc=(i == 1))
                    t1, t2, t3 = next_tmp(), next_tmp(), next_tmp()
                    fw.op(ACT, lambda: nc.scalar.activation(out=tmps[t1][:, :T], in_=banks[bA][:, :T], func=AF.Sigmoid,
                                                            bias=cv("ba", oc), scale=1.0),
                          reads=[("B", bA)] + CONST_KEYS, writes=[("tmp", t1)])
                    fw.op(ACT, lambda: nc.scalar.activation(out=tmps[t2][:, :T], in_=banks[bX][:, :T], func=AF.Sigmoid,
                                                            bias=cv("bx", oc), scale=1.0),
                          reads=[("B", bX)] + CONST_KEYS, writes=[("tmp", t2)])
                    fw.op(ACT, lambda: nc.scalar.activation(out=tmps[t3][:, :T], in_=tmps[t1][:, :T], func=AF.Exp,
                                                            scale=cv("cl2", oc)),
                          reads=[("tmp", t1)] + CONST_KEYS, writes=[("tmp", t3)])
                    fw.op(ACT, lambda: nc.scalar.activation(out=tmps[t1][:, :T], in_=tmps[t1][:, :T], func=AF.Exp,
                                                            scale=cv("cl", oc)),
                          reads=[("tmp", t1)] + CONST_KEYS, writes=[("tmp", t1)])
                    fw.op(ACT, lambda: nc.scalar.activation(out=tmps[t3][:, :T], in_=tmps[t3][:, :T], func=AF.Relu,
                                                            bias=1.0, scale=-1.0),
                          reads=[("tmp", t3)], writes=[("tmp", t3)])
                    fw.op(ACT, lambda: nc.scalar.activation(out=tmps[t3][:, :T], in_=tmps[t3][:, :T], func=AF.Sqrt),
                          reads=[("tmp", t3)], writes=[("tmp", t3)])
                    fw.op(DVE, lambda: nc.vector.tensor_tensor(out=tmps[t2][:, :T], in0=tmps[t2][:, :T],
                                                               in1=C[:, oc, :T], op=ALU.mult),
                          reads=[("tmp", t2), ck(oc)], writes=[("tmp", t2)])
                    fw.op(DVE, lambda: nc.vector.tensor_tensor(out=tmps[t2][:, :T], in0=tmps[t2][:, :T],
                                                               in1=tmps[t3][:, :T], op=ALU.mult),
                          reads=[("tmp", t2), ("tmp", t3)], writes=[("tmp", t2)])
                    for (si_, col0, ln) in segs:
                        fw.op(DVE, lambda si_=si_, col0=col0, ln=ln:
                              nc.vector.tensor_tensor_scan(out=tmps[t3][:, col0:col0 + ln], data0=tmps[t1][:, col0:col0 + ln],
                                                           data1=tmps[t2][:, col0:col0 + ln], initial=ST[si_][:, oc, 3:4],
                                                           op0=ALU.mult, op1=ALU.add),
                              reads=[("tmp", t1), ("tmp", t2), ("tmp", t3), ("ST", si_)], writes=[("tmp", t3)])
                        fw.op(ACT, lambda si_=si_, col0=col0, ln=ln:
                              nc.scalar.copy(out=ST[si_][:, oc, 3:4], in_=tmps[t3][:, col0 + ln - 1:col0 + ln]),
                              reads=[("tmp", t3), ("ST", si_)], writes=[("ST", si_)])
                    fw.op(DVE, lambda: nc.vector.tensor_tensor(out=xb[:, oc, :T], in0=tmps[t3][:, :T], in1=G[:, oc, :T],
                                                               op=ALU.mult),
                          reads=[("tmp", t3), ("G", oc)], writes=[("xb", oc)])
        for g in range(4):
            def evac_o(bks, g=g):
                for oi, bk in enumerate(bks):
                    oc = g * 4 + oi
                    fw.op(DVE, lambda: nc.vector.scalar_tensor_tensor(out=Xc[:, oc, :T], in0=Xc[:, oc, :T], scalar=ALPHA,
                                                                      in1=banks[bk][:, :T], op0=ALU.mult, op1=ALU.add),
                          reads=[("B", bk), xk(oc)], writes=[xk(oc)])
                    ln_acc(Xc, xk, oc, T, first=(oc == 0))
            stream_group("rg_out", T, NCH, g, rhs, rkeys, evac_o)
        ln_apply(Xc, xk, T, "mixg0", "mixb0",
                 [(AF.Identity, lambda c: Xc[:, c, :T], xk), (AF.Identity, lambda c: xb[:, c, :T], lambda c: ("xb", c))])

        for hf in range(2):
            expert(T, xi, "ffn_in", hf * 11, "ffn_out", hf * 4, None, first=(hf == 0), ln_final=(hf == 1))
        ln_apply(Xc, xk, T, "ffng0", "ffnb0",
                 [(AF.Identity, lambda c: Xc[:, c, :T], xk), (AF.Identity, lambda c: xb[:, c, :T], lambda c: ("xb", c))])
        ple(T, xi, 0, need_xb=True)

        rhs, rkeys = xb_rhs(T)
        for j in range(8):
            def evac_pw1(lst, j=j):
                for q in range(2):
                    c = 2 * j + q
                    bval, bgate = lst[q], lst[2 + q]
                    ti = next_tmp()
                    fw.op(ACT, lambda: nc.scalar.activation(out=tmps[ti][:, :T], in_=banks[bgate][:, :T], func=AF.Sigmoid,
                                                            bias=cv("bpw1", 16 + c), scale=1.0),
                          reads=[("B", bgate)] + CONST_KEYS, writes=[("tmp", ti)])
                    for (si_, col0, ln) in segs:
                        cbi = next_cb()
                        cb = cbufs[cbi]
                        fw.op(ACT, lambda: nc.scalar.copy(out=cb[:, 0:30], in_=ST[si_][:, c, 4:34]),
                              reads=[("ST", si_)], writes=[("cb", cbi)])
                        fw.op(DVE, lambda: nc.vector.scalar_tensor_tensor(out=cb[:, 30:30 + ln], in0=banks[bval][:, col0:col0 + ln],
                                                                          scalar=cv("bpw1", c), in1=tmps[ti][:, col0:col0 + ln],
                                                                          op0=ALU.add, op1=ALU.mult),
                              reads=[("B", bval), ("tmp", ti), ("cb", cbi)] + CONST_KEYS, writes=[("cb", cbi)])
                        y = C[:, c, col0:col0 + ln]
                        ta, tb = next_tmp(), next_tmp()
                        accs = [(y, ck(c)), (tmps[ta][:, :ln], ("tmp", ta)), (tmps[tb][:, :ln], ("tmp", tb))]
                        for k in range(31):
                            acc, akey = accs[k % 3]
                            wk = cv("dww", k=c * 31 + k)
                            if k == 0:
                                fw.op(DVE, lambda: nc.vector.tensor_scalar(out=acc, in0=cb[:, 0:ln], scalar1=wk,
                                                                           scalar2=cv("dwb", c), op0=ALU.mult, op1=ALU.add),
                                      reads=[("cb", cbi)] + CONST_KEYS, writes=[akey])
                            elif k < 3:
                                fw.op(DVE, lambda: nc.vector.tensor_scalar(out=acc, in0=cb[:, k:k + ln], scalar1=wk,
                                                                           scalar2=None, op0=ALU.mult),
                                      reads=[("cb", cbi)] + CONST_KEYS, writes=[akey])
                            else:
                                fw.op(DVE, lambda: nc.vector.scalar_tensor_tensor(out=acc, in0=cb[:, k:k + ln], scalar=wk,
                                                                                  in1=acc, op0=ALU.mult, op1=ALU.add),
                                      reads=[("cb", cbi), akey] + CONST_KEYS, writes=[akey])
                        for (acc, akey) in accs[1:]:
                            fw.op(DVE, lambda: nc.vector.tensor_tensor(out=y, in0=y, in1=acc, op=ALU.add),
                                  reads=[ck(c), akey], writes=[ck(c)])
                        fw.op(ACT, lambda: nc.scalar.copy(out=ST[si_][:, c, 4:34], in_=cb[:, ln:ln + 30]),
                              reads=[("cb", cbi), ("ST", si_)], writes=[("ST", si_)])
                    ln_acc(C, ck, c, T, first=(c == 0))
            stream_group("pw1", T, NCH, j, rhs, rkeys, evac_pw1)
        ln_apply(C, ck, T, "cmg", "cmb", [(AF.Silu, lambda c: G[:, c, :T], lambda c: ("G", c))])
        grhs = lambda kc: G[:, kc, :T]
        gkeys = lambda kc: [("G", kc)]
        for g in range(4):
            def evac_pw2(bks, g=g):
              for oi, bk in enumerate(bks):
                oc = g * 4 + oi
                ti = next_tmp()
                fw.op(ACT, lambda: nc.scalar.activation(out=tmps[ti][:, :T], in_=banks[bk][:, :T], func=AF.Identity,
                                                        bias=cv("bpw2", oc), scale=1.0),
                      reads=[("B", bk)] + CONST_KEYS, writes=[("tmp", ti)])
                fw.op(DVE, lambda: nc.vector.scalar_tensor_tensor(out=Xc[:, oc, :T], in0=Xc[:, oc, :T], scalar=ALPHA,
                                                                  in1=tmps[ti][:, :T], op0=ALU.mult, op1=ALU.add),
                      reads=[("tmp", ti), xk(oc)], writes=[xk(oc)])
                ln_acc(Xc, xk, oc, T, first=(oc == 0))
            stream_group("pw2", T, NCH, g, grhs, gkeys, evac_pw2)
        if t["last"]:
            for (si_, _, _) in segs:
                if t["kind"] == "p":
                    dst = st_out_p[t["b"]]
                else:
                    dst = st_out_s[si_]
                fw.dma(SP, s_so, dst, ST[si_][:], reads=[("ST", si_)])
        if ti_ + 1 < len(tiles):
            tn = tiles[ti_ + 1]
            fw.dma(SP, s_x, C[:, :, :tn["T"]], x_src(tn), writes=[ck(c) for c in range(NCH)])
        ln_apply(Xc, xk, T, "mixg1", "mixb1",
                 [(AF.Identity, lambda c: Xc[:, c, :T], xk), (AF.Identity, lambda c: xb[:, c, :T], lambda c: ("xb", c))])

        rows = min(128, T)
        ntb = max(1, T // 128)
        o_wr, _ = _cv["wr"]
        bL = next_banks(1)[0]
        for tb in range(ntb):
            for c in range(NCH):
                fw.op(PE, lambda tb=tb, c=c: nc.tensor.matmul(banks[bL][0:rows, tb * NE:(tb + 1) * NE],
                                                              lhsT=Xc[:, c, tb * 128:tb * 128 + rows],
                                                              rhs=cvec[:, o_wr + c * NE:o_wr + (c + 1) * NE],
                                                              start=(c == 0), stop=(c == NCH - 1)),
                      reads=[xk(c)] + CONST_KEYS, writes=[("B", bL)], inc=(c == NCH - 1))
        fw.op(ACT, lambda: nc.scalar.copy(out=rt[0:rows, 0:ntb, :], in_=banks[bL][0:rows, 0:ntb * NE].rearrange("p (a e) -> p a e", e=NE)),
              reads=[("B", bL)], writes=[("rt",)])
        for tb in range(ntb):
            L = rt[0:rows, tb, :]
            L2 = rt2[0:rows, tb, :]
            Gt = rg[0:rows, tb, :]
            m1 = rs[0:rows, tb, 0:1]
            m2 = rs[0:rows, tb, 1:2]
            ssum = rs[0:rows, tb, 2:3]
            RK = [("rt",), ("rt2",), ("rg",), ("rs",)]
            dv = lambda fn: fw.op(DVE, fn, reads=RK, writes=RK[1:])
            dv(lambda: nc.vector.tensor_reduce(out=m1, in_=L, axis=mybir.AxisListType.X, op=ALU.max))
            dv(lambda: nc.vector.tensor_scalar(out=L2, in0=L, scalar1=m1, scalar2=None, op0=ALU.is_equal))
            dv(lambda: nc.vector.scalar_tensor_tensor(out=L2, in0=L2, scalar=-1e30, in1=L, op0=ALU.mult, op1=ALU.add))
            dv(lambda: nc.vector.tensor_reduce(out=m2, in_=L2, axis=mybir.AxisListType.X, op=ALU.max))
            dv(lambda: nc.vector.tensor_scalar(out=L2, in0=L, scalar1=m2, scalar2=None, op0=ALU.is_ge))
            dv(lambda: nc.vector.tensor_scalar(out=Gt, in0=L, scalar1=m1, scalar2=None, op0=ALU.subtract))
            fw.op(ACT, lambda: nc.scalar.activation(out=Gt, in_=Gt, func=AF.Exp), reads=RK, writes=RK[1:])
            dv(lambda: nc.vector.tensor_tensor(out=Gt, in0=Gt, in1=L2, op=ALU.mult))
            dv(lambda: nc.vector.tensor_reduce(out=ssum, in_=Gt, axis=mybir.AxisListType.X, op=ALU.add))
            dv(lambda: nc.vector.reciprocal(out=ssum, in_=ssum))
            dv(lambda: nc.vector.tensor_scalar(out=Gt, in0=Gt, scalar1=ssum, scalar2=None, op0=ALU.mult))
        bT = next_banks(1)[0]
        for tb in range(ntb):
            fw.op(PE, lambda tb=tb: nc.tensor.transpose(banks[bT][0:NE, tb * 128:tb * 128 + rows], rg[0:rows, tb, :],
                                                        ident[0:rows, 0:rows]),
                  reads=[("rg",), ("ident",)], writes=[("B", bT)])
        fw.op(ACT, lambda: nc.scalar.copy(out=gT[:, :T], in_=banks[bT][0:NE, :T]), reads=[("B", bT)], writes=[("gT",)])
        for e in range(NE):
            expert(T, xi, "moe_in", e * 11, "moe_out", e * 4, e, first=(e == 0), ln_final=(e == NE - 1))
        need_xb1 = False
        ln_apply(Xc, xk, T, "ffng1", "ffnb1",
                 [(AF.Identity, lambda c: Xc[:, c, :T], xk), (AF.Identity, lambda c: xb[:, c, :T], lambda c: ("xb", c))])
        ple(T, xi, 1, need_xb=need_xb1)
        def _store(t=t, Xc=Xc, T=T, xk=xk):
            fw.dma(SP, s_y, y_dst(t), Xc[:, :, :T], reads=[xk(c) for c in range(NCH)])
        if ti_ + 1 < len(tiles):
            after_wload.append(_store)
        else:
            _store()

    fw._emit_waits(SP, [(s_y.sem, s_y.count, "y"), (s_so.sem, s_so.count, "so")])
    es.close()
    return nc, fw


def _fm(v):
    v = np.asarray(v, np.float32)
    lead = v.shape[:-1]
    a = v.reshape(lead + (NCH, 128))
    return np.moveaxis(a, -1, 0)


def _build_cvec(inp):
    tab = np.zeros((128, NV), np.float32)

    def put(name, arr):
        o, w = _cv[name]
        tab[:, o:o + w] = np.asarray(arr, np.float32).reshape(128, w)

    put("convw", np.transpose(_fm(inp["rglru_conv_w"][0]), (0, 2, 1)))
    put("convb", _fm(inp["rglru_conv_b"][0]))
    put("ba", _fm(inp["rglru_b_a"][0]))
    put("bx", _fm(inp["rglru_b_x"][0]))
    put("lam", _fm(inp["rglru_lambda"][0]))
    put("bpw1", np.moveaxis(np.asarray(inp["cm_b_pw1"][0], np.float32).reshape(32, 128), -1, 0))
    put("dww", np.transpose(_fm(inp["cm_dw_w"][0]), (0, 2, 1)))
    put("dwb", _fm(inp["cm_dw_b"][0]))
    put("cmg", _fm(inp["cm_ln_g"][0]))
    put("cmb", _fm(inp["cm_ln_b"][0]))
    put("bpw2", _fm(inp["cm_b_pw2"][0]))
    for i in range(2):
        put(f"mixg{i}", _fm(inp["ln_mix_g"][i]))
        put(f"mixb{i}", _fm(inp["ln_mix_b"][i]))
        put(f"ffng{i}", _fm(inp["ln_ffn_g"][i]))
        put(f"ffnb{i}", _fm(inp["ln_ffn_b"][i]))
        put(f"pleb{i}", _fm(inp["ple_b_gate"][i]))
    wr = np.asarray(inp["moe_w_router"][0], np.float32).reshape(NCH, 128, NE)
    put("wr", np.transpose(wr, (1, 0, 2)))
    return tab


_PROG_CACHE = {}


def kernel(**inp):
    tile_sel = None
    if os.environ.get("MK_TILES"):
        tile_sel = [int(s) for s in os.environ["MK_TILES"].split(",")]
    key = tuple(tile_sel) if tile_sel else None
    if key not in _PROG_CACHE:
        _PROG_CACHE[key] = build_program(tile_sel)
    nc, fw = _PROG_CACHE[key]

    f32 = lambda a: np.asarray(a, np.float32)
    xp = f32(inp["x_prompt"])
    xs = f32(inp["x_sample"])
    pp = f32(inp["p_prompt"])
    ps = f32(inp["p_sample"])
    cvec = _build_cvec(inp)
    ident = np.eye(128, dtype=np.float32)
    selmat = np.zeros((NE, NE * 128), np.float32)
    for e in range(NE):
        selmat[e, e * 128:(e + 1) * 128] = 1.0
    shared = {
        "cvec": cvec, "ident": ident, "selmat": selmat,
        "rglru_w_in": f32(inp["rglru_w_in"][0]),
        "rglru_w_a": f32(inp["rglru_w_a"][0]).reshape(D, 256),
        "rglru_w_x": f32(inp["rglru_w_x"][0]).reshape(D, 256),
        "rglru_w_out": f32(inp["rglru_w_out"][0]),
        "cm_w_pw1": f32(inp["cm_w_pw1"][0]),
        "cm_w_pw2": f32(inp["cm_w_pw2"][0]),
        "ffn_w_in": f32(inp["ffn_w_in"][0]),
        "ffn_w_out": f32(inp["ffn_w_out"][0]),
        "moe_w_in": f32(inp["moe_w_in"][0]).reshape(NE * D, 2 * EFF),
        "moe_w_out": f32(inp["moe_w_out"][0]).reshape(NE * EFF, D),
        "ple_w_proj0": f32(inp["ple_w_proj"][0]), "ple_w_proj1": f32(inp["ple_w_proj"][1]),
        "ple_w_gate0": f32(inp["ple_w_gate"][0]), "ple_w_gate1": f32(inp["ple_w_gate"][1]),
    }
    st_tok = np.concatenate([f32(inp["state_rglru_conv"][0]), f32(inp["state_rglru_h"][0])[:, None, :],
                             f32(inp["state_conv_module"][0])], axis=1)
    st_fm = np.ascontiguousarray(st_tok.reshape(16, NST, NCH, 128).transpose(0, 3, 2, 1))
    in_maps = []
    for c in range(NCORES):
        b0 = 2 * c
        xTp = np.ascontiguousarray(xp[b0:b0 + 2].reshape(2, SEQ, NCH, 128).transpose(0, 3, 2, 1))
        xTs = np.ascontiguousarray(xs[b0:b0 + 2].reshape(2 * DEC, NCH, 128).transpose(2, 1, 0))
        pTp = np.ascontiguousarray(pp[:, b0:b0 + 2].reshape(2, 2, SEQ, 2, 128).transpose(0, 1, 4, 3, 2))
        pTs = np.ascontiguousarray(ps[:, b0:b0 + 2].reshape(2, 2 * DEC, 2, 128).transpose(0, 3, 2, 1))
        m = dict(shared)
        m.update({"xTp": xTp, "xTs": xTs, "pTp": pTp, "pTs": pTs, "st_in": st_fm[b0:b0 + 2]})
        in_maps.append(m)
    res = run_bass_kernel_spmd(nc, in_maps, core_ids=list(range(NCORES)))
    R = res.results
    yTp = np.stack([r["yTp"] for r in R])
    y_prompt = np.ascontiguousarray(yTp.transpose(0, 1, 4, 3, 2)).reshape(16, SEQ, D)
    yTs = np.stack([r["yTs"] for r in R])
    y_sample = np.ascontiguousarray(yTs.transpose(0, 3, 2, 1)).reshape(16, DEC, D)

    def split_state(key):
        s = np.stack([r[key] for r in R]).reshape(16, 128, NCH, NST)
        tok = np.ascontiguousarray(s.transpose(0, 3, 2, 1)).reshape(16, NST, D)
        return (np.ascontiguousarray(tok[None, :, 0:3, :]), np.ascontiguousarray(tok[None, :, 3, :]),
                np.ascontiguousarray(tok[None, :, 4:34, :]))
    cp, hp, cmp_ = split_state("st_out_p")
    cs, hs, cms = split_state("st_out_s")
    return (y_prompt, y_sample, cp, hp, cmp_, cs, hs, cms)
```
